# Optimizing a Trainium2 kernel written in Bass

```python
import math
import jax, jax.numpy as jnp
from jax import lax
import numpy as np

D_MODEL = 1024
BATCH = 32
SEQ = 2048
DEPTH = 1

CHUNK = 64
D_CONV = 1024
CONV_WIDTH = 3
HEAD_DIM = 64
N_HEADS = 16
D_ATT = N_HEADS * HEAD_DIM
Q_BLOCK = 128
N_GROUPS = 4
EXPERTS_PER_GROUP = 8
N_EXPERTS = N_GROUPS * EXPERTS_PER_GROUP
TOP_K = 2
D_EXPERT = 512
MOE_BLOCK = 256
EPS = 1e-6

SPLITS = (D_CONV, D_CONV, D_CONV, D_ATT, D_ATT, D_ATT, N_HEADS, D_MODEL, D_MODEL)
D_IN_PROJ = sum(SPLITS)

kernel_name = "hybrid_conv_fox_hiermoe_block"


def rmsnorm(x, g):
    xf = x.astype(jnp.float32)
    y = xf * lax.rsqrt(jnp.mean(xf * xf, axis=-1, keepdims=True) + EPS)
    return (y * g.astype(jnp.float32)).astype(x.dtype)


def split_cols(p):
    idx = np.cumsum(np.array(SPLITS))[:-1].tolist()
    return jnp.split(p, idx, axis=-1)


def short_conv(b, c, u, conv_w, conv_b):
    S = u.shape[1]
    z = c * u
    zp = jnp.pad(z, ((0, 0), (CONV_WIDTH - 1, 0), (0, 0)))
    acc = conv_b
    for i in range(CONV_WIDTH):
        acc = acc + conv_w[i] * zp[:, i:i + S]
    return b * acc


def forgetting_attention(q, k, v, f_logit, b_forget):
    B, S, H, dh = q.shape
    log_f = jax.nn.log_sigmoid(f_logit.astype(jnp.float32) + b_forget.astype(jnp.float32))
    cum = jnp.transpose(jnp.cumsum(log_f, axis=1), (0, 2, 1))
    scale = 1.0 / math.sqrt(dh)
    outs = []
    for qb in range(S // Q_BLOCK):
        qs, qe = qb * Q_BLOCK, (qb + 1) * Q_BLOCK
        qblk = q[:, qs:qe]
        kp, vp = k[:, :qe], v[:, :qe]
        s = jnp.einsum('bqhd,bkhd->bhqk', qblk, kp).astype(jnp.float32) * scale
        s = s + (cum[:, :, qs:qe, None] - cum[:, :, None, :qe])
        t_pos = jnp.arange(qs, qe)[:, None]
        s_pos = jnp.arange(qe)[None, :]
        s = jnp.where(s_pos <= t_pos, s, -jnp.inf)
        p = jax.nn.softmax(s, axis=-1).astype(v.dtype)
        outs.append(jnp.einsum('bhqk,bkhd->bqhd', p, vp))
    return jnp.concatenate(outs, axis=1)


def token_mixer(h, w_in, conv_w, conv_b, b_forget, w_conv_out, w_att_out, w_out):
    B, S, _ = h.shape
    proj = h @ w_in
    cb, cc, cu, q, k, v, f_logit, g_conv, g_att = split_cols(proj)
    y_conv = short_conv(cb, cc, cu, conv_w, conv_b) @ w_conv_out
    o = forgetting_attention(q.reshape(B, S, N_HEADS, HEAD_DIM),
                             k.reshape(B, S, N_HEADS, HEAD_DIM),
                             v.reshape(B, S, N_HEADS, HEAD_DIM),
                             f_logit, b_forget)
    y_att = o.reshape(B, S, D_ATT) @ w_att_out
    m = jax.nn.sigmoid(g_conv) * y_conv + jax.nn.sigmoid(g_att) * y_att
    return m @ w_out


def hier_moe(h, w_router_group, w_router_expert, w_e_gate, w_e_up, w_e_down):
    B, S, D = h.shape
    T = B * S
    hf = h.reshape(T, D)
    pg = jax.nn.softmax((hf @ w_router_group).astype(jnp.float32), axis=-1)
    g_val, g_idx = lax.top_k(pg, 1)
    le = jnp.einsum('td,gde->tge', hf, w_router_expert).astype(jnp.float32)
    le_sel = jnp.take_along_axis(le, g_idx[:, :, None], axis=1)[:, 0]
    pe = jax.nn.softmax(le_sel, axis=-1)
    e_val, e_idx = lax.top_k(pe, TOP_K)
    e_val = e_val / jnp.sum(e_val, axis=-1, keepdims=True)
    w_tok = (g_val * e_val).astype(h.dtype)
    expert = (g_idx * EXPERTS_PER_GROUP + e_idx).astype(jnp.int32)

    A = T * TOP_K
    flat_e = expert.reshape(A)
    flat_w = w_tok.reshape(A)
    flat_tok = jnp.arange(A, dtype=jnp.int32) // TOP_K
    order = jnp.argsort(flat_e)
    sorted_e = flat_e[order]
    counts = jnp.bincount(flat_e, length=N_EXPERTS)
    padded = (counts + MOE_BLOCK - 1) // MOE_BLOCK * MOE_BLOCK
    start_sorted = jnp.cumsum(counts) - counts
    end_padded = jnp.cumsum(padded)
    start_padded = end_padded - padded
    rank = jnp.arange(A, dtype=jnp.int32) - start_sorted[sorted_e]
    dest = start_padded[sorted_e] + rank
    n_rows = A + N_EXPERTS * MOE_BLOCK
    n_blocks = n_rows // MOE_BLOCK
    row_tok = jnp.zeros((n_rows,), jnp.int32).at[dest].set(flat_tok[order])
    row_w = jnp.zeros((n_rows,), h.dtype).at[dest].set(flat_w[order])
    block_e = jnp.minimum(
        jnp.searchsorted(end_padded, jnp.arange(n_blocks) * MOE_BLOCK, side='right'),
        N_EXPERTS - 1).astype(jnp.int32)
    x_rows = hf[row_tok].reshape(n_blocks, MOE_BLOCK, D)

    def expert_block(args):
        xb, e = args
        a = xb @ w_e_gate[e]
        u = xb @ w_e_up[e]
        return (jax.nn.silu(a) * u) @ w_e_down[e]

    y_rows = lax.map(expert_block, (x_rows, block_e)).reshape(n_rows, D)
    y = jax.ops.segment_sum(y_rows * row_w[:, None], row_tok, num_segments=T)
    return y.reshape(B, S, D)


def setup_inputs(seed: int = 0) -> dict:
    key = jax.random.key(seed)
    ks = jax.random.split(key, 20)
    f32 = jnp.float32
    nrm = lambda k, shape, fan: jax.random.normal(k, shape, f32) * (fan ** -0.5)
    return {
        "x": jax.random.normal(ks[0], (BATCH, SEQ, D_MODEL), f32),
        "norm_mix_g": 1.0 + 0.02 * jax.random.normal(ks[1], (D_MODEL,), f32),
        "w_in": nrm(ks[2], (D_MODEL, D_IN_PROJ), D_MODEL),
        "conv_w": nrm(ks[3], (CONV_WIDTH, D_CONV), CONV_WIDTH),
        "conv_b": 0.01 * jax.random.normal(ks[4], (D_CONV,), f32),
        "b_forget": jax.random.uniform(ks[5], (N_HEADS,), f32, 1.0, 6.0),
        "w_conv_out": nrm(ks[6], (D_CONV, D_MODEL), D_CONV),
        "w_att_out": nrm(ks[7], (D_ATT, D_MODEL), D_ATT),
        "w_out": nrm(ks[8], (D_MODEL, D_MODEL), D_MODEL),
        "norm_ffn_g": 1.0 + 0.02 * jax.random.normal(ks[9], (D_MODEL,), f32),
        "w_router_group": nrm(ks[10], (D_MODEL, N_GROUPS), D_MODEL),
        "w_router_expert": nrm(ks[11], (N_GROUPS, D_MODEL, EXPERTS_PER_GROUP), D_MODEL),
        "w_e_gate": nrm(ks[12], (N_EXPERTS, D_MODEL, D_EXPERT), D_MODEL),
        "w_e_up": nrm(ks[13], (N_EXPERTS, D_MODEL, D_EXPERT), D_MODEL),
        "w_e_down": nrm(ks[14], (N_EXPERTS, D_EXPERT, D_MODEL), D_EXPERT),
        "norm_final_g": 1.0 + 0.02 * jax.random.normal(ks[15], (D_MODEL,), f32),
    }


def reference(x, norm_mix_g, w_in, conv_w, conv_b, b_forget, w_conv_out, w_att_out, w_out,
              norm_ffn_g, w_router_group, w_router_expert, w_e_gate, w_e_up, w_e_down,
              norm_final_g):
    for _ in range(DEPTH):
        x = x + token_mixer(rmsnorm(x, norm_mix_g), w_in, conv_w, conv_b, b_forget,
                            w_conv_out, w_att_out, w_out)
        x = x + hier_moe(rmsnorm(x, norm_ffn_g), w_router_group, w_router_expert,
                         w_e_gate, w_e_up, w_e_down)
    return rmsnorm(x, norm_final_g)
```

```python
import numpy as np
from contextlib import ExitStack
import concourse.bass as bass
import concourse.mybir as mybir
from concourse.bass_utils import run_bass_kernel_spmd

F32 = mybir.dt.float32
BF16 = mybir.dt.bfloat16
I32 = mybir.dt.int32
ALU = mybir.AluOpType
AF = mybir.ActivationFunctionType
AX = mybir.AxisListType

D = 1024
FILL = False
NH = 16
NE = 32
DE = 512
DIN = 8208
EPS = 1e-6
MB = 256


class Op:
    __slots__ = ("eng", "fn", "reads", "writes", "stream", "idx", "waits", "signal", "ticket", "bar")

    def __init__(self, eng, fn, reads, writes, stream, bar=False):
        self.eng = eng
        self.fn = fn
        self.reads = reads
        self.writes = writes
        self.stream = stream
        self.waits = {}
        self.signal = False
        self.ticket = None
        self.bar = bar


class Prog:
    ENGS = ("pe", "act", "dve", "pool", "sp")

    def __init__(self, nc):
        self.nc = nc
        self.ops = []

    def op(self, eng, fn, reads=(), writes=(), stream=None):
        o = Op(eng, fn, tuple(reads), tuple(writes), stream)
        o.idx = len(self.ops)
        self.ops.append(o)
        return o

    def dma(self, eng, out, in_, reads=(), writes=(), stream="ld", **kw):
        return self.op(eng, lambda e: e.dma_start(out=out, in_=in_, **kw), reads, writes, stream=stream)

    def barrier(self):
        for en in self.ENGS:
            o = Op(en, lambda e: e.nop(), (), (), None, bar=True)
            o.idx = len(self.ops)
            self.ops.append(o)

    def build(self):
        ops = self.ops
        last_w, readers = {}, {}
        last_on_eng = {}
        deps = [None] * len(ops)
        bar_streams = [None] * len(ops)
        run = {}
        run_at = [None] * len(ops)
        for o in ops:
            run_at[o.idx] = dict(run)
            d = set()
            if o.bar:
                for en, li in last_on_eng.items():
                    d.add(li)
                bar_streams[o.idx] = dict(run)
            else:
                for r in o.reads:
                    if r in last_w:
                        d.add(last_w[r])
                for w in o.writes:
                    if w in last_w:
                        d.add(last_w[w])
                    for rd in readers.get(w, ()):
                        d.add(rd)
                for r in o.reads:
                    readers.setdefault(r, []).append(o.idx)
                for w in o.writes:
                    last_w[w] = o.idx
                    readers[w] = []
            d.discard(o.idx)
            deps[o.idx] = d
            if o.stream is not None:
                run[o.stream] = run.get(o.stream, 0) + 1
            else:
                last_on_eng[o.eng] = o.idx
            if o.bar:
                pass
        needed = [dict() for _ in ops]
        for o in ops:
            for p in deps[o.idx]:
                po = ops[p]
                if po.stream is None and po.eng == "pe" and o.eng == "pe" and o.stream is None and not o.bar:
                    continue
                if po.stream is None:
                    po.signal = True
                    dm = ("eng", po.eng)
                else:
                    dm = ("dma", po.stream)
                if dm not in needed[o.idx] or needed[o.idx][dm] < p:
                    needed[o.idx][dm] = p
            if o.bar:
                for s in bar_streams[o.idx]:
                    needed[o.idx][("dma", s)] = -1
        ecount = {}
        for o in ops:
            if o.stream is None and o.signal:
                ecount[o.eng] = ecount.get(o.eng, 0) + 1
                o.ticket = ecount[o.eng]
        waited = {e: {} for e in self.ENGS}
        for o in ops:
            w = {}
            for dm, p in needed[o.idx].items():
                if dm[0] == "dma":
                    val = 16 * run_at[o.idx].get(dm[1], 0)
                else:
                    val = ops[p].ticket
                if val == 0 or waited[o.eng].get(dm, 0) >= val:
                    continue
                waited[o.eng][dm] = val
                w[dm] = val
            o.waits = w
        doms = []
        for o in ops:
            dm = ("dma", o.stream) if o.stream is not None else ("eng", o.eng)
            if dm not in doms and (o.signal or o.stream is not None):
                doms.append(dm)
        self.run_total = run
        return doms

    def emit(self, st, out_streams=()):
        nc = self.nc
        doms = self.build()
        sems = {}
        for dm in doms:
            sems[dm] = st.enter_context(nc.semaphore("s_" + "_".join(str(x) for x in dm)))
        block = st.enter_context(nc.Block())
        ops = self.ops
        run_total = self.run_total

        def make(engname):
            def body(e):
                for o in ops:
                    if o.eng != engname:
                        continue
                    for dm, val in o.waits.items():
                        e.wait_ge(sems[dm], val)
                    ins = o.fn(e)
                    if o.stream is not None:
                        ins.then_inc(sems[("dma", o.stream)], 16)
                    elif o.signal:
                        ins.then_inc(sems[("eng", engname)], 1)
                if engname == "sp":
                    for s in out_streams:
                        e.wait_ge(sems[("dma", s)], 16 * run_total[s])
            return body

        block.tensor(make("pe"))
        block.scalar(make("act"))
        block.vector(make("dve"))
        block.gpsimd(make("pool"))
        block.sync(make("sp"))


def build_program(NSEQ, debug=False):
    nc = bass.Bass("TRN2", target_bir_lowering=False)
    TOK = NSEQ * 2048
    NT = TOK // 128
    NROWS = 2 * TOK + NE * MB
    NBLK = NROWS // MB

    def din(name, shape, dt=F32):
        return nc.dram_tensor(name, list(shape), dt, kind="ExternalInput").ap()

    def dscr(name, shape, dt, out=False):
        return nc.dram_tensor(name, list(shape), dt, kind=("ExternalOutput" if out else "Internal")).ap()

    x = din("x", [TOK, D])
    g1 = din("norm_mix_g", [D]); g2 = din("norm_ffn_g", [D]); g3 = din("norm_final_g", [D])
    w_in = din("w_in", [D, DIN])
    conv_w = din("conv_w", [3, D]); conv_b = din("conv_b", [D]); b_forget = din("b_forget", [NH])
    w_co = din("w_conv_out", [D, D]); w_ao = din("w_att_out", [D, D]); w_o = din("w_out", [D, D])
    w_rg = din("w_router_group", [D, 4]); w_re = din("w_router_expert", [4, D, 8])
    w_eg = din("w_e_gate", [NE, D, DE]); w_eu = din("w_e_up", [NE, D, DE]); w_ed = din("w_e_down", [NE, DE, D])
    consts = din("consts", [128, 6 * 128])
    out = nc.dram_tensor("out", [TOK, D], F32, kind="ExternalOutput").ap()

    win_bf = dscr("win_bf", [128, 8, DIN], BF16)
    wsq_bf = dscr("wsq_bf", [3, 128, 8, D], BF16)
    wall_bf = dscr("wall_bf", [NE * 128, 12288], BF16)
    x1s = dscr("x1s", [TOK, D], F32, out=debug)
    h2s = dscr("h2s", [TOK, D], BF16)
    xs = dscr("xs", [NROWS, D], BF16)
    ys = dscr("ys", [NROWS, D], BF16)

    st = ExitStack()
    with st:
        off = [16544]

        def sb(name, shape, dt, at=None):
            nbytes = int(np.prod(shape[1:])) * (4 if dt in (F32, I32) else 2)
            nbytes = (nbytes + 31) // 32 * 32
            if at is None:
                o = off[0]
                off[0] += nbytes
            else:
                o = at[0]
                at[0] += nbytes
            assert o + nbytes <= 229344, (name, o, nbytes)
            return nc.alloc_sbuf_tensor_at(name, list(shape), dt, offset=o)

        psum = [st.enter_context(nc.psum_tensor(f"ps{i}", [128, 512], F32)) for i in range(6)]
        psT = [st.enter_context(nc.psum_tensor(f"psT{i}", [128, 1024], BF16)) for i in range(2)]

        P = Prog(nc)
        cst = sb("cst", [128, 6 * 128], F32)
        ident_bf = sb("ident_bf", [128, 128], BF16)
        tri_bf = sb("tri_bf", [128, 128], BF16)
        ustr_bf = sb("ustr_bf", [128, 128], BF16)
        ones_bf = sb("ones_bf", [128, 128], BF16)
        tri_f = cst[:, 128:256]
        e127_f = cst[:, 384:512]
        iota_p = cst[:, 640:641]
        thr = cst[:, 641:641 + 96]
        cwT = sb("cwT", [128, 3, 8], F32)
        cbT = sb("cbT", [128, 8], F32)
        bfb = sb("bfb", [128, NH], F32)
        wr_bf = sb("wr_bf", [128, 8, 36], BF16)
        logits = sb("logits", [128, NT, 36], F32)
        gb = sb("gb", [128, D], F32)
        gb2 = sb("gb2", [128, D], F32)
        P.dma("sp", cst[:], consts, writes=["cst"], stream="m_cst")
        P.op("dve", lambda e: e.tensor_copy(out=ident_bf[:], in_=cst[:, 0:128]), ["cst"], ["ident"])
        P.op("dve", lambda e: e.tensor_copy(out=tri_bf[:], in_=cst[:, 128:256]), ["cst"], ["tri"])
        P.op("dve", lambda e: e.tensor_copy(out=ustr_bf[:], in_=cst[:, 256:384]), ["cst"], ["ustr"])
        P.op("dve", lambda e: e.tensor_copy(out=ones_bf[:], in_=cst[:, 512:640]), ["cst"], ["ones"])
        with nc.allow_non_contiguous_dma(reason="tiny per-channel params"):
            P.dma("sp", cwT[:], conv_w.rearrange("t (c p) -> p t c", p=128), writes=["cwT"], allow_slow_non_contiguous=True, stream="m_cw")
            P.dma("sp", cbT[:], conv_b.rearrange("(c p) -> p c", p=128), writes=["cbT"], allow_slow_non_contiguous=True, stream="m_cb")
        P.dma("sp", bfb[:], b_forget.partition_broadcast(128), writes=["bfb"], stream="m_bf")
        P.dma("sp", gb[:], g1.partition_broadcast(128), writes=["gb"], stream="m_gb")
        P.dma("sp", gb2[:], g2.partition_broadcast(128), writes=["gb2"], stream="m_gb2")
        base = off[0]

        at = [base]
        stgA = [sb(f"stg{i}", [128, 8, 512], F32, at) for i in range(4)]
        stbA = [sb(f"stb{i}", [128, 8, 512], BF16, at) for i in range(4)]
        cnt = [0]

        def convert(src_ap, dst_ap, kk, ncol, eng_cast, stq="pool", wres="wscr", stg=None, stb=None, tag="a"):
            stg = stg or stgA
            stb = stb or stbA
            i = cnt[0] % len(stg)
            cnt[0] += 1
            P.dma("sp", stg[i][:, 0:kk, 0:ncol], src_ap, reads=[], writes=[f"stg{tag}{i}"], stream=f"cv{tag}{i}")
            if eng_cast == "act":
                P.op("act", lambda e: e.copy(out=stb[i][:, 0:kk, 0:ncol], in_=stg[i][:, 0:kk, 0:ncol]), [f"stg{tag}{i}"], [f"stb{tag}{i}"])
            else:
                P.op(eng_cast, lambda e: e.tensor_copy(out=stb[i][:, 0:kk, 0:ncol], in_=stg[i][:, 0:kk, 0:ncol]), [f"stg{tag}{i}"], [f"stb{tag}{i}"])
            P.dma(stq, dst_ap, stb[i][:, 0:kk, 0:ncol], reads=[f"stb{tag}{i}"], writes=[(wres, cnt[0])], stream=f"wst{tag}{i}")

        NZ = NROWS // 256
        engs = ["dve", "act", "pool"]
        ci = 0
        for c0 in range(0, DIN, 512):
            nco = min(512, DIN - c0)
            convert(w_in[:, c0:c0 + nco].rearrange("(k p) n -> p k n", p=128), win_bf[:, :, c0:c0 + nco], 8, nco, engs[ci % 3]); ci += 1
        for mi, wm in enumerate((w_co, w_ao, w_o)):
            for c0 in range(0, D, 512):
                convert(wm[:, c0:c0 + 512].rearrange("(k p) n -> p k n", p=128), wsq_bf[mi, :, :, c0:c0 + 512], 8, 512, engs[ci % 3]); ci += 1
        wr_f = sb("wr_f", [128, 8, 36], F32, at)
        with nc.allow_non_contiguous_dma(reason="tiny router weights"):
            P.dma("sp", wr_f[:, :, 0:4], w_rg.rearrange("(k p) n -> p k n", p=128), writes=["wr_f0"], allow_slow_non_contiguous=True, stream="m_wr")
            for g in range(4):
                P.dma("sp", wr_f[:, :, 4 + 8 * g:12 + 8 * g], w_re[g].rearrange("(k p) n -> p k n", p=128), writes=[f"wr_f{g + 1}"], allow_slow_non_contiguous=True, stream="m_wr")
        P.op("dve", lambda e: e.tensor_copy(out=wr_bf[:], in_=wr_f[:]), [f"wr_f{i}" for i in range(5)], ["wr_bf"])
        P.barrier()

        at = [base]
        wbuf = [sb(f"wbuf{i}", [128, 8, 512], BF16, at) for i in range(3)]
        hT = sb("hT", [128, 8, 2048], BF16, at)
        attT = sb("attT", [128, 8, 2048], BF16, at)
        wf_bf = sb("wf_bf", [128, 8, 16], BF16, at)
        xt = [sb(f"xt{i}", [128, D], F32, at) for i in range(2)]
        hbf = [sb(f"hbf{i}", [128, D], BF16, at) for i in range(2)]
        junk = sb("junk", [128, D], BF16, at)
        stat = sb("stat", [128, 8], F32, at)
        spall = sb("spall", [128, 16, NH], F32, at)
        cumneg = sb("cumneg", [128, 16, NH], F32, at)
        crefB = sb("crefB", [128, 16, NH], F32, at)
        ftmp = sb("ftmp", [128, NH], F32, at)
        ftmp4 = sb("ftmp4", [128, 4, NH], F32, at)
        baseX = at[0]
        vaug = [sb(f"vaug{i}", [128, 16, 2, 128], BF16, at) for i in range(2)]
        qTp = [sb(f"qTp{i}", [128, 2, 2048], BF16, at) for i in range(2)]
        kTp = [sb(f"kTp{i}", [128, 2048], BF16, at) for i in range(2)]
        wqk = [sb(f"wqk{i}", [128, 8, 3, 128], BF16, at) for i in range(2)]
        biasT = [sb(f"biasT{i}", [128, 16, 16], F32, at) for i in range(2)]
        PT = [sb(f"PT{i}", [128, 512], BF16, at) for i in range(6)]
        rden = [sb(f"rden{i}", [128, 512], F32, at) for i in range(2)]
        cvg = [sb(f"cvg{i}", [128, 2, 512], F32, at) for i in range(2)]
        cvb = [sb(f"cvb{i}", [128, 2, 512], BF16, at) for i in range(2)]
        zt = sb("zt", [128, 2048], BF16, at)
        zstate = {"n": 0}
        ZPER = (NZ + NSEQ * 8 - 1) // (NSEQ * 8)

        def zero_fill_step():
            for _ in range(ZPER):
                zi = zstate["n"]
                if zi < NZ:
                    P.dma("sp", xs[zi * 256:(zi + 1) * 256, :].rearrange("(p j) d -> p (j d)", j=2), zt[:], reads=["zt"], writes=[("xsz", zi)], stream="zx")
                    zstate["n"] = zi + 1
        endA12 = at[0]
        cjobs = []
        for ex in range(NE):
            rows = wall_bf[ex * 128:(ex + 1) * 128, :]
            for wsrc, c0 in ((w_eg, 0), (w_eu, 4096)):
                sv = wsrc[ex].rearrange("(k p) n -> p k n", p=128)
                dv = rows[:, c0:c0 + 4096].rearrange("p (k n) -> p k n", k=8)
                for q in range(4):
                    cjobs.append((sv[:, 2 * q:2 * q + 2, :], dv[:, 2 * q:2 * q + 2, :]))
            sv = w_ed[ex].rearrange("(k p) n -> p k n", p=128)
            dv = rows[:, 8192:12288].rearrange("p (k n) -> p k n", k=4)
            for k in range(4):
                cjobs.append((sv[:, k, :].rearrange("p (a n) -> p a n", a=2), dv[:, k, :].rearrange("p (a n) -> p a n", a=2)))
        cst8 = {"next": 0, "pend": None}

        def conv_finish():
            m = cst8["pend"]
            if m is not None:
                i = m % 2
                eng = "dve"
                P.op(eng, lambda e, i=i: e.tensor_copy(out=cvb[i][:], in_=cvg[i][:]), [f"cvg{i}"], [f"cvb{i}"])
                P.dma("sp", cjobs[m][1], cvb[i][:], reads=[f"cvb{i}"], writes=[("wex", m)], stream=f"cvs{i}")
                cst8["pend"] = None

        def conv_step():
            m = cst8["next"]
            old = cst8["pend"]
            if m < len(cjobs):
                i = m % 2
                P.dma("sp", cvg[i][:], cjobs[m][0], writes=[f"cvg{i}"], stream=f"cvl{i}")
            conv_finish()
            if m < len(cjobs):
                cst8["pend"] = m
                cst8["next"] = m + 1
        CONV_PER_PAIR = (len(cjobs) + NSEQ * 8 - 1) // (NSEQ * 8)
        at = [baseX]
        zT = sb("zT", [128, 8, 516], BF16, at)
        cS = [sb(f"cS{i}", [128, 512], F32, at) for i in range(2)]
        acc = [sb(f"acc{i}", [128, 512], F32, at) for i in range(2)]
        cvT = sb("cvT", [128, 8, 512], BF16, at)
        sgc = sb("sgc", [128, 8, 512], BF16, at)
        sga = sb("sga", [128, 8, 512], BF16, at)
        t1 = [sb(f"t1{i}", [128, 512], F32, at) for i in range(2)]
        mT = sb("mT", [128, 8, 512], BF16, at)
        xr = [sb(f"xr{i}", [128, D], F32, at) for i in range(2)]
        x1t = [sb(f"x1t{i}", [128, D], F32, at) for i in range(2)]
        h2t = [sb(f"h2t{i}", [128, D], BF16, at) for i in range(2)]
        h2T = [sb(f"h2T{i}", [128, 8, 128], BF16, at) for i in range(2)]
        junk3 = sb("junk3", [128, D], BF16, at)
        stat3 = sb("stat3", [128, 8], F32, at)
        endA3 = at[0]

        inv_d = 1.0 / D

        def rstd_ops(sumsq_ap, out_ap, rn, wn):
            P.op("dve", lambda e: e.tensor_scalar(out=out_ap, in0=sumsq_ap, scalar1=inv_d, scalar2=EPS, op0=ALU.mult, op1=ALU.add), rn, wn)
            P.op("act", lambda e: e.activation(out=out_ap, in_=out_ap, func=AF.Sqrt), wn, wn)
            P.op("dve", lambda e: e.reciprocal(out=out_ap, in_=out_ap), wn, wn)

        COLS = {"b": 0, "c": 1024, "u": 2048, "q": 3072, "k": 4096, "v": 5120, "f": 6144, "gc": 6160, "ga": 7184}
        wcount = [0]

        wplan = []
        for _ch in range(4):
            for half in range(2):
                for nm in ("c", "u", "b"):
                    wplan.append(win_bf[:, :, COLS[nm] + half * 512:COLS[nm] + (half + 1) * 512])
            for nm in ("gc", "ga"):
                for half in range(2):
                    wplan.append(win_bf[:, :, COLS[nm] + half * 512:COLS[nm] + (half + 1) * 512])
            for half in range(2):
                wplan.append(wsq_bf[0, :, :, half * 512:(half + 1) * 512])
                wplan.append(wsq_bf[1, :, :, half * 512:(half + 1) * 512])
            wplan.append(wsq_bf[2, :, :, 0:512])
            wplan.append(wsq_bf[2, :, :, 512:1024])
        wstate = {"cur": 0, "issued": 0}

        def w_issue(g):
            i = g % 3
            P.dma("sp", wbuf[i][:], wplan[g], reads=["wscr"], writes=[f"wbuf{i}"], stream=f"ldw{i}")

        def w_reset():
            wstate["cur"] = 0
            wstate["issued"] = 0
            for g in range(3):
                w_issue(g)
            wstate["issued"] = 3

        def load_w(src_ap=None):
            g = wstate["cur"]
            wstate["cur"] += 1
            return wbuf[g % 3], f"wbuf{g % 3}"

        def w_done(n=1):
            for _ in range(n):
                g = wstate["issued"]
                if g < len(wplan):
                    w_issue(g)
                    wstate["issued"] += 1

        pcount = [0]

        def next_ps():
            i = pcount[0] % 6
            pcount[0] += 1
            return psum[i], f"ps{i}"

        P.dma("sp", wf_bf[:], win_bf[:, :, COLS["f"]:COLS["f"] + 16], reads=["wscr"], writes=["wf_bf"], stream="ldwf")


        def a1_tiles(s, tiles):
            t0 = s * 2048
            tiles = list(tiles)

            def stA(tt):
                i = tt % 2
                s0, s1 = stat[:, 2 * i:2 * i + 1], stat[:, 2 * i + 1:2 * i + 2]
                P.dma("sp", xt[i][:], x[t0 + tt * 128:t0 + (tt + 1) * 128, :], writes=[f"xt{i}"], stream=f"ldxt{i}")
                P.op("act", lambda e, i=i, s0=s0: e.activation(out=junk[:], in_=xt[i][:], func=AF.Square, accum_out=s0),
                     [f"xt{i}"], ["junk", f"stat0{i}"])
                rstd_ops(s0, s1, [f"stat0{i}"], [f"stat1{i}"])
                P.op("dve", lambda e, i=i, s1=s1: e.scalar_tensor_tensor(out=hbf[i][:], in0=xt[i][:], scalar=s1, in1=gb[:], op0=ALU.mult, op1=ALU.mult),
                     [f"xt{i}", f"stat1{i}", "gb"], [f"hbf{i}"])

            def stB(tt):
                i = tt % 2
                pt, ptn = psT[tt % 2], f"psT{tt % 2}"
                def tr(e, i=i, pt=pt):
                    for k in range(8):
                        ins = e.transpose(out=pt[:, k * 128:(k + 1) * 128], in_=hbf[i][:, k * 128:(k + 1) * 128], identity=ident_bf[:])
                    return ins
                P.op("pe", tr, [f"hbf{i}", "ident"], [ptn])
                P.op("act", lambda e, pt=pt, tt=tt: e.copy(out=hT[:, :, tt * 128:(tt + 1) * 128], in_=pt[:].rearrange("p (k t) -> p k t", k=8)),
                     [ptn], [("hT", tt)])

            def stC(group):
                ps, psn = next_ps()
                def fl(e, ps=ps, group=group):
                    for gi, tt in enumerate(group):
                        for k in range(8):
                            ins = e.matmul(ps[:, gi * 16:(gi + 1) * 16], lhsT=hT[:, k, tt * 128:(tt + 1) * 128], rhs=wf_bf[:, k, :], start=(k == 0), stop=(k == 7))
                    return ins
                P.op("pe", fl, [("hT", tt) for tt in group] + ["wf_bf"], [psn])
                n = len(group)
                g0 = group[0]
                fv = ftmp4[:, 0:n, :]
                P.op("dve", lambda e, ps=ps, n=n, fv=fv: e.tensor_tensor(out=fv, in0=ps[:, 0:16 * n].rearrange("p (a b) -> p a b", a=n), in1=bfb[:].unsqueeze(1).to_broadcast([128, n, NH]), op=ALU.add), [psn, "bfb"], ["ftmp"])
                P.op("act", lambda e, fv=fv: e.activation(out=fv, in_=fv, func=AF.Exp, scale=-1.0), ["ftmp"], ["ftmp"])
                P.op("act", lambda e, fv=fv, g0=g0, n=n: e.activation(out=spall[:, g0:g0 + n, :], in_=fv, func=AF.Ln, bias=1.0), ["ftmp"], [("sp", tt) for tt in group])

            n = len(tiles)
            for idx in range(n + 1):
                if idx < n:
                    stA(tiles[idx])
                if idx >= 1:
                    stB(tiles[idx - 1])
            for g in range(0, n, 4):
                stC(tiles[g:g + 4])

        def a1_finish(s):
            for tt in range(16):
                ps, psn = next_ps()
                def cm(e, ps=ps, tt=tt):
                    ins = e.matmul(ps[:, 0:16], lhsT=tri_f, rhs=spall[:, tt, :], start=True, stop=(tt == 0))
                    if tt > 0:
                        ins = e.matmul(ps[:, 0:16], lhsT=e127_f, rhs=cumneg[:, tt - 1, :], start=False, stop=True)
                    return ins
                P.op("pe", cm, [("sp", tt), "cst"] + ([("cum", tt - 1)] if tt else []), [psn])
                P.op("dve", lambda e, ps=ps, tt=tt: e.tensor_copy(out=cumneg[:, tt, :], in_=ps[:, 0:16]), [psn], [("cum", tt)])
            ps, psn = next_ps()
            P.op("pe", lambda e, ps=ps: e.matmul(ps[:, 0:256], lhsT=e127_f, rhs=cumneg[:].rearrange("p a b -> p (a b)"), start=True, stop=True),
                 [("cum", tt) for tt in range(16)] + ["cst"], [psn])
            P.op("dve", lambda e, ps=ps: e.tensor_copy(out=crefB[:].rearrange("p a b -> p (a b)"), in_=ps[:, 0:256]), [psn], ["crefB"])


        a1_tiles(0, range(16))
        a1_finish(0)
        for s in range(NSEQ):
            t0 = s * 2048
            for j in range(8):
                pb = j % 2
                if j == 0:
                    P.op("pool", lambda e: e.memset(zt[:], 0.0), [], ["zt"])
                zero_fill_step()
                for wi, nm in enumerate(("q", "k", "v")):
                    P.dma("sp", wqk[pb][:, :, wi, :], win_bf[:, :, COLS[nm] + j * 128:COLS[nm] + (j + 1) * 128], reads=["wscr"], writes=[f"wqk{pb}_{wi}"], stream=f"ldq{pb}_{wi}")
                if j < 2:
                    P.op("pool", lambda e, pb=pb: e.memset(qTp[pb][:, 0, :], 0.0), [], [(f"qTp{pb}", c) for c in range(4)])
                    P.op("pool", lambda e, pb=pb: e.memset(qTp[pb][:, 1, :], 0.0), [], [(f"qTp{pb}", c) for c in range(4)])
                for wi, dst, dn in ((0, qTp[pb], f"qTp{pb}"), (1, kTp[pb], f"kTp{pb}")):
                    for ch in range(4):
                        ps, psn = next_ps()
                        def pj(e, ps=ps, ch=ch, wi=wi, pb=pb):
                            for k in range(8):
                                ins = e.matmul(ps[:], lhsT=wqk[pb][:, k, wi, :], rhs=hT[:, k, ch * 512:(ch + 1) * 512], start=(k == 0), stop=(k == 7))
                            return ins
                        P.op("pe", pj, [f"wqk{pb}_{wi}"] + [("hT", tt) for tt in range(ch * 4, ch * 4 + 4)], [psn])
                        if wi == 0:
                            P.op("act", lambda e, ps=ps, dst=dst, ch=ch: e.copy(out=dst[0:64, 0, ch * 512:(ch + 1) * 512], in_=ps[0:64, :]), [psn], [(dn, ch)])
                            P.op("dve", lambda e, ps=ps, dst=dst, ch=ch: e.tensor_copy(out=dst[64:128, 1, ch * 512:(ch + 1) * 512], in_=ps[64:128, :]), [psn], [(dn, ch)])
                        elif ch % 2 == 0:
                            P.op("act", lambda e, ps=ps, dst=dst, ch=ch: e.copy(out=dst[:, ch * 512:(ch + 1) * 512], in_=ps[:]), [psn], [(dn, ch)])
                        else:
                            P.op("dve", lambda e, ps=ps, dst=dst, ch=ch: e.tensor_copy(out=dst[:, ch * 512:(ch + 1) * 512], in_=ps[:]), [psn], [(dn, ch)])
                if j < 2:
                    P.op("pool", lambda e, pb=pb: e.memset(vaug[pb][:].rearrange("p a b c -> p (a b c)"), 1.0), [], [(f"vaug{pb}", g) for g in range(4)])
                for g4 in range(4):
                    ps, psn = next_ps()
                    def pv(e, ps=ps, g4=g4, pb=pb):
                        for q4 in range(4):
                            tt = g4 * 4 + q4
                            for k in range(8):
                                ins = e.matmul(ps[:, q4 * 128:(q4 + 1) * 128], lhsT=hT[:, k, tt * 128:(tt + 1) * 128], rhs=wqk[pb][:, k, 2, :], start=(k == 0), stop=(k == 7))
                        return ins
                    P.op("pe", pv, [f"wqk{pb}_2"] + [("hT", tt) for tt in range(g4 * 4, g4 * 4 + 4)], [psn])
                    psv = ps[:].rearrange("p (t h d) -> p t h d", t=4, h=2)
                    P.op("dve", lambda e, psv=psv, g4=g4, pb=pb: e.tensor_copy(out=vaug[pb][:, g4 * 4:(g4 + 1) * 4, 0, 0:64], in_=psv[:, :, 0, :]), [psn], [(f"vaug{pb}", g4)])
                    P.op("dve", lambda e, psv=psv, g4=g4, pb=pb: e.tensor_copy(out=vaug[pb][:, g4 * 4:(g4 + 1) * 4, 1, 64:128], in_=psv[:, :, 1, :]), [psn], [(f"vaug{pb}", g4)])
                groups = []
                for hh in range(2):
                    h = 2 * j + hh
                    bi = hh
                    def bt(e, bi=bi, h=h):
                        for qb in range(16):
                            ins = e.tensor_scalar(out=biasT[bi][:, 0:qb + 1, qb], in0=cumneg[:, 0:qb + 1, h], scalar1=crefB[:, 2 * (qb // 2), h:h + 1], scalar2=None, op0=ALU.subtract)
                        return ins
                    P.op("pool", bt, [("cum", tt) for tt in range(16)] + ["crefB"], [f"biasT{bi}"])
                    for qc in range(4):
                        for kb in range(4 * qc + 4):
                            groups.append((hh, qc, kb))
                LA = 4
                SB5 = [(psum[3], "ps3"), (psum[4], "ps4"), (psum[5], "ps5"), (psT[0][:].bitcast(F32), "psT0"), (psT[1][:].bitcast(F32), "psT1")]

                def emit_qk(t, pb=pb):
                    hh, qc, kb = groups[t]
                    lo, hi = 64 * hh, 64 * hh + 64
                    off = 128 * max(0, kb - 4 * qc)
                    pss, pssn = SB5[t % 5]
                    P.op("pe", lambda e: e.matmul(pss[:, off:512], lhsT=kTp[pb][:, kb * 128:(kb + 1) * 128],
                                                  rhs=qTp[pb][:, hh, qc * 512 + off:(qc + 1) * 512], start=True, stop=True),
                         [(f"qTp{pb}", qc), (f"kTp{pb}", kb // 4)], [pssn])

                def emit_rest(t, pb=pb, j=j):
                    hh, qc, kb = groups[t]
                    bi = hh
                    jj = max(0, kb - 4 * qc)
                    off = 128 * jj
                    pss, pssn = SB5[t % 5]
                    oi = (hh * 4 + qc) % 3
                    pso, pson = psum[oi], f"ps{oi}"
                    ptb, ptn2 = PT[t % 6], f"PT{t % 6}"
                    def ex(e):
                        for sp in range(2):
                            c_lo = max(off, 256 * sp)
                            c_hi = 256 * sp + 256
                            if c_lo >= c_hi:
                                continue
                            qb = 4 * qc + 2 * sp
                            ins = e.activation(out=ptb[:, c_lo:c_hi], in_=pss[:, c_lo:c_hi], func=AF.Exp,
                                               bias=biasT[bi][:, kb, qb + 1:qb + 2] if kb > qb else biasT[bi][:, kb, qb:qb + 1], scale=0.125)
                        return ins
                    P.op("act", ex, [pssn, f"biasT{bi}"], [ptn2])
                    if kb >= 4 * qc:
                        P.op("pool", lambda e: e.tensor_tensor(out=ptb[:, off:off + 128], in0=ptb[:, off:off + 128], in1=tri_bf[:], op=ALU.mult),
                             [ptn2, "tri"], [ptn2])
                    last = (kb == 4 * qc + 3)
                    P.op("pe", lambda e: e.matmul(pso[:, off:512], lhsT=vaug[pb][:, kb, hh, :], rhs=ptb[:, off:512], start=(kb == 0), stop=last),
                         [ptn2, (f"vaug{pb}", kb // 4)], [pson])
                    if FILL:
                        fps = psT[1][:].bitcast(F32)
                        P.op("pe", lambda e: e.matmul(fps[:, 0:512], lhsT=ident_bf[:], rhs=hT[:, 0, 0:512], start=True, stop=True), [], ["psT1"])
                    if last:
                        ri = qc % 2
                        cs = slice(qc * 512, (qc + 1) * 512)
                        if hh == 0:
                            P.op("dve", lambda e: e.reciprocal(out=rden[ri][0:64, :], in_=pso[64:128, :]), [pson], [f"rden{ri}"])
                            P.op("dve", lambda e: e.tensor_tensor(out=attT[0:64, j, cs], in0=pso[0:64, :], in1=rden[ri][0:64, :], op=ALU.mult),
                                 [pson, f"rden{ri}"], [("attT", j, qc, 0)])
                        else:
                            P.op("dve", lambda e: e.reciprocal(out=rden[ri][64:128, :], in_=pso[0:64, :]), [pson], [f"rden{ri}"])
                            P.op("dve", lambda e: e.tensor_tensor(out=attT[64:128, j, cs], in0=pso[64:128, :], in1=rden[ri][64:128, :], op=ALU.mult),
                                 [pson, f"rden{ri}"], [("attT", j, qc, 1)])

                cdone = 0
                for t in range(len(groups) + LA):
                    if t < len(groups):
                        emit_qk(t)
                    if t >= LA:
                        emit_rest(t - LA)
                    if t % 6 == 5 and cdone < CONV_PER_PAIR:
                        conv_step()
                        cdone += 1
                while cdone < CONV_PER_PAIR:
                    conv_step()
                    cdone += 1
                if j == 7:
                    conv_finish()
            w_reset()
            P.barrier()

            P.op("pool", lambda e: e.memset(zT[:, :, 0:2], 0.0), [], ["zT"])
            for ch in range(4):
                c0 = ch * 512
                hTr = [("hT", tt) for tt in range(ch * 4, ch * 4 + 4)]
                for half in range(2):
                    wbs = {}
                    for fi in range(4):
                        fc = half * 4 + fi
                    wb, wbn = load_w(win_bf[:, :, COLS["c"] + half * 512:COLS["c"] + (half + 1) * 512])
                    cS_list = []
                    for fi in range(4):
                        fc = half * 4 + fi
                        ps, psn = next_ps()
                        def mm(e, ps=ps, wb=wb, fi=fi, c0=c0):
                            for k in range(8):
                                ins = e.matmul(ps[:], lhsT=wb[:, k, fi * 128:(fi + 1) * 128], rhs=hT[:, k, c0:c0 + 512], start=(k == 0), stop=(k == 7))
                            return ins
                        P.op("pe", mm, [wbn] + hTr, [psn])
                        P.op("act", lambda e, ps=ps, fc=fc: e.copy(out=mT[:, fc, :], in_=ps[:]), [psn], [("mTc", fc), ("mT", fc)])
                    w_done()
                    wb, wbn = load_w(win_bf[:, :, COLS["u"] + half * 512:COLS["u"] + (half + 1) * 512])
                    for fi in range(4):
                        fc = half * 4 + fi
                        ps, psn = next_ps()
                        def mm(e, ps=ps, wb=wb, fi=fi, c0=c0):
                            for k in range(8):
                                ins = e.matmul(ps[:], lhsT=wb[:, k, fi * 128:(fi + 1) * 128], rhs=hT[:, k, c0:c0 + 512], start=(k == 0), stop=(k == 7))
                            return ins
                        P.op("pe", mm, [wbn] + hTr, [psn])
                        P.op("dve", lambda e, ps=ps, fc=fc: e.tensor_tensor(out=zT[:, fc, 2:514], in0=ps[:], in1=mT[:, fc, :], op=ALU.mult), [psn, ("mTc", fc), "zT"], [("zT", fc)])
                    w_done()
                    wb, wbn = load_w(win_bf[:, :, COLS["b"] + half * 512:COLS["b"] + (half + 1) * 512])
                    for fi in range(4):
                        fc = half * 4 + fi
                        ps, psn = next_ps()
                        def mm(e, ps=ps, wb=wb, fi=fi, c0=c0):
                            for k in range(8):
                                ins = e.matmul(ps[:], lhsT=wb[:, k, fi * 128:(fi + 1) * 128], rhs=hT[:, k, c0:c0 + 512], start=(k == 0), stop=(k == 7))
                            return ins
                        P.op("pe", mm, [wbn] + hTr, [psn])
                        ai = fc % 2
                        P.op("pool", lambda e, fc=fc, ai=ai: e.tensor_scalar(out=acc[ai][:], in0=zT[:, fc, 2:514], scalar1=cwT[:, 2, fc:fc + 1], scalar2=cbT[:, fc:fc + 1], op0=ALU.mult, op1=ALU.add),
                             [("zT", fc), "cwT", "cbT"], [f"acc{ai}"])
                        P.op("dve", lambda e, fc=fc, ai=ai: e.scalar_tensor_tensor(out=acc[ai][:], in0=zT[:, fc, 1:513], scalar=cwT[:, 1, fc:fc + 1], in1=acc[ai][:], op0=ALU.mult, op1=ALU.add),
                             [("zT", fc), f"acc{ai}"], [f"acc{ai}"])
                        P.op("dve", lambda e, fc=fc, ai=ai: e.scalar_tensor_tensor(out=acc[ai][:], in0=zT[:, fc, 0:512], scalar=cwT[:, 0, fc:fc + 1], in1=acc[ai][:], op0=ALU.mult, op1=ALU.add),
                             [("zT", fc), f"acc{ai}"], [f"acc{ai}"])
                        P.op("dve", lambda e, ps=ps, fc=fc, ai=ai: e.tensor_tensor(out=cvT[:, fc, :], in0=ps[:], in1=acc[ai][:], op=ALU.mult), [psn, f"acc{ai}"], [("cvT", fc)])
                        P.op("pool", lambda e, fc=fc: e.tensor_copy(out=zT[:, fc, 0:2], in_=zT[:, fc, 512:514]), [("zT", fc), f"acc{ai}"], [("zT", fc)])
                    w_done()
                for (nm, dstg, dgn) in (("gc", sgc, "sgc"), ("ga", sga, "sga")):
                    for half in range(2):
                        wb, wbn = load_w(win_bf[:, :, COLS[nm] + half * 512:COLS[nm] + (half + 1) * 512])
                        for fi in range(4):
                            fc = half * 4 + fi
                            ps, psn = next_ps()
                            def mm(e, ps=ps, wb=wb, fi=fi, c0=c0):
                                for k in range(8):
                                    ins = e.matmul(ps[:], lhsT=wb[:, k, fi * 128:(fi + 1) * 128], rhs=hT[:, k, c0:c0 + 512], start=(k == 0), stop=(k == 7))
                                return ins
                            P.op("pe", mm, [wbn] + hTr, [psn])
                            P.op("act", lambda e, ps=ps, dstg=dstg, fc=fc: e.activation(out=dstg[:, fc, :], in_=ps[:], func=AF.Sigmoid), [psn], [(dgn, fc)])
                        w_done()
                for half in range(2):
                    wbc, wbcn = load_w(wsq_bf[0, :, :, half * 512:(half + 1) * 512])
                    wba, wban = load_w(wsq_bf[1, :, :, half * 512:(half + 1) * 512])
                    for fi in range(4):
                        fo = half * 4 + fi
                        ps1, ps1n = next_ps()
                        def mm1(e, ps=ps1, wb=wbc, fi=fi):
                            for k in range(8):
                                ins = e.matmul(ps[:], lhsT=wb[:, k, fi * 128:(fi + 1) * 128], rhs=cvT[:, k, :], start=(k == 0), stop=(k == 7))
                            return ins
                        P.op("pe", mm1, [wbcn] + [("cvT", k) for k in range(8)], [ps1n])
                        ps2, ps2n = next_ps()
                        def mm2(e, ps=ps2, wb=wba, fi=fi, c0=c0):
                            for k in range(8):
                                ins = e.matmul(ps[:], lhsT=wb[:, k, fi * 128:(fi + 1) * 128], rhs=attT[:, k, c0:c0 + 512], start=(k == 0), stop=(k == 7))
                            return ins
                        P.op("pe", mm2, [wban] + [("attT", k, ch, hh) for k in range(8) for hh in range(2)], [ps2n])
                        ti = fo % 2
                        P.op("dve", lambda e, ps=ps1, fo=fo, ti=ti: e.tensor_tensor(out=t1[ti][:], in0=ps[:], in1=sgc[:, fo, :], op=ALU.mult), [ps1n, ("sgc", fo)], [f"t1{ti}"])
                        P.op("dve", lambda e, ps=ps2, fo=fo, ti=ti: e.tensor_tensor(out=acc[ti][:], in0=ps[:], in1=sga[:, fo, :], op=ALU.mult), [ps2n, ("sga", fo)], [f"acc{ti}"])
                        P.op("pool", lambda e, fo=fo, ti=ti: e.tensor_tensor(out=mT[:, fo, :], in0=t1[ti][:], in1=acc[ti][:], op=ALU.add), [f"t1{ti}", f"acc{ti}", ("mTc", fo)], [("mT", fo), ("mTc", fo)])
                    w_done(2)
                wo0, wo0n = load_w(wsq_bf[2, :, :, 0:512])
                wo1, wo1n = load_w(wsq_bf[2, :, :, 512:1024])
                def stX(q4):
                    tt = ch * 4 + q4
                    gt = s * 16 + tt
                    i = tt % 2
                    P.dma("act", xr[i][:], x[t0 + tt * 128:t0 + (tt + 1) * 128, :], writes=[f"xr{i}"], stream=f"ldxr{i}")
                    for half, (wo, won) in enumerate(((wo0, wo0n), (wo1, wo1n))):
                        ps, psn = next_ps()
                        def mm(e, ps=ps, wo=wo, q4=q4):
                            for k in range(8):
                                ins = e.matmul(ps[:], lhsT=mT[:, k, q4 * 128:(q4 + 1) * 128], rhs=wo[:, k, :], start=(k == 0), stop=(k == 7))
                            return ins
                        P.op("pe", mm, [won] + [("mT", k) for k in range(8)], [psn])
                        P.op("dve", lambda e, ps=ps, i=i, half=half: e.tensor_tensor(out=x1t[i][:, half * 512:(half + 1) * 512], in0=ps[:], in1=xr[i][:, half * 512:(half + 1) * 512], op=ALU.add),
                             [psn, f"xr{i}"], [(f"x1t{i}", half)])
                    P.dma("pool", x1s[t0 + tt * 128:t0 + (tt + 1) * 128, :], x1t[i][:], reads=[(f"x1t{i}", 0), (f"x1t{i}", 1)], writes=[("x1s", gt)], stream=f"stx{i}")

                def stY(q4):
                    tt = ch * 4 + q4
                    gt = s * 16 + tt
                    i = tt % 2
                    s0, s1 = stat3[:, 2 * i:2 * i + 1], stat3[:, 2 * i + 1:2 * i + 2]
                    P.op("act", lambda e, i=i, s0=s0: e.activation(out=junk3[:], in_=x1t[i][:], func=AF.Square, accum_out=s0),
                         [(f"x1t{i}", 0), (f"x1t{i}", 1)], ["junk3", f"s30{i}"])
                    rstd_ops(s0, s1, [f"s30{i}"], [f"s31{i}"])
                    P.op("dve", lambda e, i=i, s1=s1: e.scalar_tensor_tensor(out=h2t[i][:], in0=x1t[i][:], scalar=s1, in1=gb2[:], op0=ALU.mult, op1=ALU.mult),
                         [(f"x1t{i}", 0), (f"x1t{i}", 1), f"s31{i}", "gb2"], [f"h2t{i}"])
                    P.dma("pool", h2s[t0 + tt * 128:t0 + (tt + 1) * 128, :], h2t[i][:], reads=[f"h2t{i}"], writes=[("h2s", gt)], stream=f"sth{i}")

                def stZ(q4):
                    tt = ch * 4 + q4
                    gt = s * 16 + tt
                    i = tt % 2
                    pt, ptn = psT[tt % 2], f"psT{tt % 2}"
                    def tr(e, i=i, pt=pt):
                        for k in range(8):
                            ins = e.transpose(out=pt[:, k * 128:(k + 1) * 128], in_=h2t[i][:, k * 128:(k + 1) * 128], identity=ident_bf[:])
                        return ins
                    P.op("pe", tr, [f"h2t{i}", "ident"], [ptn])
                    P.op("act", lambda e, pt=pt, i=i: e.copy(out=h2T[i][:], in_=pt[:].rearrange("p (k t) -> p k t", k=8)), [ptn], [f"h2T{i}"])
                    ps, psn = next_ps()
                    def rl(e, ps=ps, i=i):
                        for k in range(8):
                            ins = e.matmul(ps[:, 0:36], lhsT=h2T[i][:, k, :], rhs=wr_bf[:, k, :], start=(k == 0), stop=(k == 7))
                        return ins
                    P.op("pe", rl, [f"h2T{i}", "wr_bf"], [psn])
                    P.op("dve", lambda e, ps=ps, gt=gt: e.tensor_copy(out=logits[:, gt, :], in_=ps[:, 0:36]), [psn], ["logits"])

                stX(0)
                stX(1)
                stY(0)
                stY(1)
                stZ(0)
                stX(2)
                stZ(1)
                stY(2)
                stX(3)
                stZ(2)
                stY(3)
                stZ(3)
                w_done(2)
                if s + 1 < NSEQ:
                    a1_tiles(s + 1, range(ch * 4, ch * 4 + 4))
            if s + 1 < NSEQ:
                a1_finish(s + 1)
            P.barrier()

        at = [base]
        NA = NT
        lg = logits[:, :, 0:4]
        le = logits[:, :, 4:36].rearrange("p t (g e) -> p t g e", g=4)
        wk = sb("wk", [128, 2, NA], F32, at)
        desti = sb("desti", [128, 2, NA], I32, at)
        idxi = sb("idxi", [128, NBLK], I32, at)
        baseB = at[0]
        gmax = sb("gmax", [128, NA], F32, at)
        ohg = sb("ohg", [128, NA, 4], F32, at)
        eg = sb("eg", [128, NA, 4], F32, at)
        gsum = sb("gsum", [128, NA], F32, at)
        gval = sb("gval", [128, NA], F32, at)
        lsel = sb("lsel", [128, NA, 8], F32, at)
        ltmp = sb("ltmp", [128, NA, 8], F32, at)
        m1 = sb("m1", [128, NA], F32, at)
        m2 = sb("m2", [128, NA], F32, at)
        oh1 = sb("oh1", [128, NA, 8], F32, at)
        oh2 = sb("oh2", [128, NA, 8], F32, at)
        M1 = sb("M1", [128, NA, 4, 8], F32, at)
        M2 = sb("M2", [128, NA, 4, 8], F32, at)
        Mb = sb("Mb", [128, NA, 32], BF16, at)
        within = sb("within", [128, NA, 32], F32, at)
        tsum = sb("tsum", [128, NA, 32], F32, at)
        carry = sb("carry", [128, NA + 1, 32], F32, at)
        prod = sb("prod", [128, NA, 32], F32, at)
        rk = sb("rk", [128, 2, NA], F32, at)
        nb = sb("nb", [128, 32], F32, at)
        cmpb = sb("cmpb", [128, 32, 32], F32, at)
        endp = sb("endp", [128, 32], F32, at)
        startp = sb("startp", [128, 32], F32, at)
        destf = sb("destf", [128, 2, NA], F32, at)
        cmp2 = sb("cmp2", [128, NBLK, 32], F32, at)
        bef = sb("bef", [128, NBLK], F32, at)

        R = ["logits"]
        bc = lambda ap, shape: ap.to_broadcast(shape)
        P.op("dve", lambda e: e.tensor_reduce(out=gmax[:], in_=lg, axis=AX.X, op=ALU.max), R, ["gmax"])
        P.op("dve", lambda e: e.tensor_tensor(out=eg[:], in0=lg, in1=gmax[:].unsqueeze(2).to_broadcast([128, NA, 4]), op=ALU.subtract), R + ["gmax"], ["eg"])
        P.op("dve", lambda e: e.tensor_single_scalar(out=ohg[:], in_=eg[:], scalar=0.0, op=ALU.is_ge), ["eg"], ["ohg"])
        P.op("act", lambda e: e.activation(out=eg[:], in_=eg[:], func=AF.Exp), ["eg", "ohg"], ["eg"])
        P.op("dve", lambda e: e.tensor_reduce(out=gsum[:], in_=eg[:], axis=AX.X, op=ALU.add), ["eg"], ["gsum"])
        P.op("dve", lambda e: e.reciprocal(out=gval[:], in_=gsum[:]), ["gsum"], ["gval"])
        for g in range(4):
            if g == 0:
                P.op("dve", lambda e: e.tensor_tensor(out=lsel[:], in0=le[:, :, 0, :], in1=ohg[:, :, 0:1].to_broadcast([128, NA, 8]), op=ALU.mult), R + ["ohg"], ["lsel"])
            else:
                P.op("dve", lambda e, g=g: e.tensor_tensor(out=ltmp[:], in0=le[:, :, g, :], in1=ohg[:, :, g:g + 1].to_broadcast([128, NA, 8]), op=ALU.mult), R + ["ohg", "lsel"], ["ltmp"])
                P.op("dve", lambda e: e.tensor_tensor(out=lsel[:], in0=lsel[:], in1=ltmp[:], op=ALU.add), ["ltmp", "lsel"], ["lsel"])
        P.op("dve", lambda e: e.tensor_reduce(out=m1[:], in_=lsel[:], axis=AX.X, op=ALU.max), ["lsel"], ["m1"])
        P.op("dve", lambda e: e.tensor_tensor(out=oh1[:], in0=lsel[:], in1=m1[:].unsqueeze(2).to_broadcast([128, NA, 8]), op=ALU.is_ge), ["lsel", "m1"], ["oh1"])
        P.op("dve", lambda e: e.scalar_tensor_tensor(out=ltmp[:], in0=oh1[:], scalar=-1e9, in1=lsel[:], op0=ALU.mult, op1=ALU.add), ["oh1", "lsel"], ["ltmp"])
        P.op("dve", lambda e: e.tensor_reduce(out=m2[:], in_=ltmp[:], axis=AX.X, op=ALU.max), ["ltmp"], ["m2"])
        P.op("dve", lambda e: e.tensor_tensor(out=oh2[:], in0=ltmp[:], in1=m2[:].unsqueeze(2).to_broadcast([128, NA, 8]), op=ALU.is_ge), ["ltmp", "m2"], ["oh2"])
        P.op("dve", lambda e: e.tensor_tensor(out=m2[:], in0=m2[:], in1=m1[:], op=ALU.subtract), ["m2", "m1", "oh2"], ["m2"])
        P.op("act", lambda e: e.activation(out=m2[:], in_=m2[:], func=AF.Exp), ["m2"], ["m2"])
        P.op("dve", lambda e: e.tensor_scalar(out=m2[:], in0=m2[:], scalar1=1.0, scalar2=None, op0=ALU.add), ["m2"], ["m2"])
        P.op("dve", lambda e: e.reciprocal(out=m2[:], in_=m2[:]), ["m2"], ["m2"])
        P.op("dve", lambda e: e.tensor_tensor(out=wk[:, 0, :], in0=gval[:], in1=m2[:], op=ALU.mult), ["m2", "gval"], ["wk0"])
        P.op("dve", lambda e: e.tensor_tensor(out=wk[:, 1, :], in0=gval[:], in1=wk[:, 0, :], op=ALU.subtract), ["wk0", "gval"], ["wk1"])
        for g in range(4):
            P.op("dve", lambda e, g=g: e.tensor_tensor(out=M1[:, :, g, :], in0=oh1[:], in1=ohg[:, :, g:g + 1].to_broadcast([128, NA, 8]), op=ALU.mult), ["oh1", "ohg"], ["M1"])
            P.op("dve", lambda e, g=g: e.tensor_tensor(out=M2[:, :, g, :], in0=oh2[:], in1=ohg[:, :, g:g + 1].to_broadcast([128, NA, 8]), op=ALU.mult), ["oh2", "ohg"], ["M2"])
        M1f = M1[:].rearrange("p t g e -> p t (g e)")
        M2f = M2[:].rearrange("p t g e -> p t (g e)")
        P.op("dve", lambda e: e.tensor_tensor(out=Mb[:], in0=M1f, in1=M2f, op=ALU.add), ["M1", "M2"], ["Mb"])
        for t in range(NA):
            ps, psn = next_ps()
            def rm(e, ps=ps, t=t):
                e.matmul(ps[:, 0:32], lhsT=ustr_bf[:], rhs=Mb[:, t, :], start=True, stop=True)
                return e.matmul(ps[:, 32:64], lhsT=ones_bf[:], rhs=Mb[:, t, :], start=True, stop=True)
            P.op("pe", rm, ["Mb", "ustr", "ones"], [psn])
            P.op("dve", lambda e, ps=ps, t=t: e.tensor_copy(out=within[:, t, :], in_=ps[:, 0:32]), [psn], ["within"])
            P.op("act", lambda e, ps=ps, t=t: e.copy(out=tsum[:, t, :], in_=ps[:, 32:64]), [psn], ["tsum"])
        P.op("pool", lambda e: e.memset(carry[:, 0, :], 0.0), [], ["carry"])
        def cch(e):
            for t in range(NA):
                ins = e.tensor_tensor(out=carry[:, t + 1, :], in0=carry[:, t, :], in1=tsum[:, t, :], op=ALU.add)
            return ins
        for t in range(NA):
            P.op("pool", lambda e, t=t: e.tensor_tensor(out=carry[:, t + 1, :], in0=carry[:, t, :], in1=tsum[:, t, :], op=ALU.add), ["tsum", "carry"], ["carry"])
        P.op("dve", lambda e: e.tensor_tensor(out=within[:], in0=within[:], in1=carry[:, 0:NA, :], op=ALU.add), ["within", "carry"], ["within"])
        for kk, Mf, mn in ((0, M1f, "M1"), (1, M2f, "M2")):
            P.op("dve", lambda e, Mf=Mf: e.tensor_tensor(out=prod[:], in0=within[:], in1=Mf, op=ALU.mult), ["within", mn], ["prod"])
            P.op("dve", lambda e, kk=kk: e.tensor_reduce(out=rk[:, kk, :], in_=prod[:], axis=AX.X, op=ALU.add), ["prod"], [f"rk{kk}"])
        counts = carry[:, NA, :]
        P.op("dve", lambda e: e.tensor_tensor(out=cmpb[:], in0=counts.unsqueeze(2).to_broadcast([128, 32, 32]), in1=thr[:, 0:32].unsqueeze(1).to_broadcast([128, 32, 32]), op=ALU.is_gt), ["carry", "cst"], ["cmpb"])
        P.op("dve", lambda e: e.tensor_reduce(out=nb[:], in_=cmpb[:], axis=AX.X, op=ALU.add), ["cmpb"], ["nb"])
        P.op("dve", lambda e: e.tensor_scalar(out=nb[:], in0=nb[:], scalar1=float(MB), scalar2=None, op0=ALU.mult), ["nb"], ["nb"])
        P.op("dve", lambda e: e.tensor_copy(out=endp[:, 0:1], in_=nb[:, 0:1]), ["nb"], ["endp"])
        for ex in range(1, 32):
            P.op("dve", lambda e, ex=ex: e.tensor_tensor(out=endp[:, ex:ex + 1], in0=endp[:, ex - 1:ex], in1=nb[:, ex:ex + 1], op=ALU.add), ["endp", "nb"], ["endp"])
        P.op("dve", lambda e: e.tensor_tensor(out=startp[:], in0=endp[:], in1=nb[:], op=ALU.subtract), ["endp", "nb"], ["startp"])
        for kk, Mf, mn in ((0, M1f, "M1"), (1, M2f, "M2")):
            P.op("dve", lambda e, Mf=Mf: e.tensor_tensor(out=prod[:], in0=Mf, in1=startp[:].unsqueeze(1).to_broadcast([128, NA, 32]), op=ALU.mult), ["startp", mn, f"rk{kk}"], ["prod"])
            P.op("dve", lambda e, kk=kk: e.tensor_reduce(out=destf[:, kk, :], in_=prod[:], axis=AX.X, op=ALU.add), ["prod"], [f"df{kk}"])
            P.op("dve", lambda e, kk=kk: e.tensor_tensor(out=destf[:, kk, :], in0=destf[:, kk, :], in1=rk[:, kk, :], op=ALU.add), [f"df{kk}", f"rk{kk}"], [f"df{kk}"])
        P.op("dve", lambda e: e.tensor_copy(out=desti[:], in_=destf[:]), ["df0", "df1"], ["desti"])
        P.op("dve", lambda e: e.tensor_tensor(out=cmp2[:], in0=endp[:].unsqueeze(1).to_broadcast([128, NBLK, 32]), in1=thr[:, 0:NBLK].unsqueeze(2).to_broadcast([128, NBLK, 32]), op=ALU.is_le), ["endp", "cst"], ["cmp2"])
        P.op("dve", lambda e: e.tensor_reduce(out=bef[:], in_=cmp2[:], axis=AX.X, op=ALU.add), ["cmp2"], ["bef"])
        P.op("dve", lambda e: e.tensor_scalar(out=bef[:], in0=bef[:], scalar1=31.0, scalar2=128.0, op0=ALU.min, op1=ALU.mult), ["bef"], ["bef"])
        P.op("dve", lambda e: e.tensor_scalar(out=bef[:], in0=bef[:], scalar1=iota_p, scalar2=None, op0=ALU.add), ["bef", "cst"], ["bef"])
        P.op("dve", lambda e: e.tensor_copy(out=idxi[:], in_=bef[:]), ["bef"], ["idxi"])

        assert zstate["n"] == NZ, (zstate, NZ)
        hb = [sb(f"hb{i}", [128, D], BF16, at) for i in range(4)]
        for t in range(NA):
            i = t % 4
            P.dma("sp", hb[i][:], h2s[t * 128:(t + 1) * 128, :], reads=[("h2s", t)], writes=[f"hb{i}"], stream=f"ldhb{i}")
            for kk in range(2):
                P.op("pool", lambda e, i=i, kk=kk, t=t: e.indirect_dma_start(out=xs, out_offset=bass.IndirectOffsetOnAxis(ap=desti[:, kk, t:t + 1], axis=0), in_=hb[i][:], in_offset=None),
                     [f"hb{i}", "desti"] + [("xsz", zi) for zi in range(NROWS // 256)], [("xs", t, kk)], stream=f"sc{i}")

        assert cst8["next"] == len(cjobs) and cst8["pend"] is None, (cst8, len(cjobs))
        P.barrier()
        at = [baseB]
        wall = [sb(f"wall{i}", [128, 12288], BF16, at) for i in range(3)]
        xrw = [sb(f"xrw{i}", [128, 2, D], BF16, at) for i in range(2)]
        xTb = [sb(f"xTb{i}", [128, 8, MB], BF16, at) for i in range(2)]
        gsb = [sb(f"gsb{i}", [128, MB], F32, at) for i in range(2)]
        aT = [sb(f"aT{i}", [128, 4, MB], BF16, at) for i in range(2)]
        yo = [sb(f"yo{i}", [128, 2, D], BF16, at) for i in range(2)]
        def load_xrw(b):
            i = b % 2
            P.dma("sp", xrw[i][:], xs[b * MB:(b + 1) * MB, :].rearrange("(j p) d -> p j d", p=128), reads=[("xs", tq, kq) for tq in range(NA) for kq in range(2)], writes=[f"xrw{i}"], stream=f"ldxrw{i}")
        load_xrw(0)
        load_xrw(1)
        for b in range(NBLK):
            i = b % 2
            wi3 = b % 3
            P.op("pool", lambda e, wi3=wi3, b=b: e.indirect_dma_start(out=wall[wi3][:], out_offset=None, in_=wall_bf,
                                                                    in_offset=bass.IndirectOffsetOnAxis(ap=idxi[:, b:b + 1], axis=0)),
                 ["idxi"] + [("wex", mm) for mm in range(len(cjobs))], [f"wall{wi3}"], stream=f"gw{wi3}")
            wgv = wall[wi3][:, 0:4096].rearrange("p (k n) -> p k n", k=8)
            wuv = wall[wi3][:, 4096:8192].rearrange("p (k n) -> p k n", k=8)
            wdv = wall[wi3][:, 8192:12288].rearrange("p (k n) -> p k n", k=4)
            for jj in range(2):
                pt, ptn = psT[jj], f"psT{jj}"
                def tr(e, i=i, jj=jj, pt=pt):
                    for k in range(8):
                        ins = e.transpose(out=pt[:, k * 128:(k + 1) * 128], in_=xrw[i][:, jj, k * 128:(k + 1) * 128], identity=ident_bf[:])
                    return ins
                P.op("pe", tr, [f"xrw{i}", "ident"], [ptn])
                if jj == 0:
                    P.op("act", lambda e, pt=pt, i=i, jj=jj: e.copy(out=xTb[i][:, :, jj * 128:(jj + 1) * 128], in_=pt[:].rearrange("p (k t) -> p k t", k=8)), [ptn], [(f"xTb{i}", jj)])
                else:
                    P.op("dve", lambda e, pt=pt, i=i, jj=jj: e.tensor_copy(out=xTb[i][:, :, jj * 128:(jj + 1) * 128], in_=pt[:].rearrange("p (k t) -> p k t", k=8)), [ptn], [(f"xTb{i}", jj)])
            if b + 2 < NBLK:
                load_xrw(b + 2)
            for fo in range(4):
                psg, psgn = next_ps()
                def mg(e, ps=psg, i=i, fo=fo, wgv=wgv, wuv=wuv):
                    for k in range(8):
                        ins = e.matmul(ps[:, 0:MB], lhsT=wgv[:, k, fo * 128:(fo + 1) * 128], rhs=xTb[i][:, k, :], start=(k == 0), stop=(k == 7))
                    for k in range(8):
                        ins = e.matmul(ps[:, MB:2 * MB], lhsT=wuv[:, k, fo * 128:(fo + 1) * 128], rhs=xTb[i][:, k, :], start=(k == 0), stop=(k == 7))
                    return ins
                P.op("pe", mg, [f"wall{wi3}", (f"xTb{i}", 0), (f"xTb{i}", 1)], [psgn])
                gi = fo % 2
                P.op("act", lambda e, ps=psg, gi=gi: e.activation(out=gsb[gi][:], in_=ps[:, 0:MB], func=AF.Silu), [psgn], [f"gsb{gi}"])
                P.op("dve", lambda e, ps=psg, gi=gi, i=i, fo=fo: e.tensor_tensor(out=aT[i][:, fo, :], in0=ps[:, MB:2 * MB], in1=gsb[gi][:], op=ALU.mult), [psgn, f"gsb{gi}"], [(f"aT{i}", fo)])
            for jj in range(2):
                for half in range(2):
                    ps, psn = next_ps()
                    def md(e, ps=ps, i=i, jj=jj, half=half, wdv=wdv):
                        for k in range(4):
                            ins = e.matmul(ps[:], lhsT=aT[i][:, k, jj * 128:(jj + 1) * 128], rhs=wdv[:, k, half * 512:(half + 1) * 512], start=(k == 0), stop=(k == 3))
                        return ins
                    P.op("pe", md, [f"wall{wi3}"] + [(f"aT{i}", k) for k in range(4)], [psn])
                    if half == 0:
                        P.op("act", lambda e, ps=ps, i=i, jj=jj, half=half: e.copy(out=yo[i][:, jj, half * 512:(half + 1) * 512], in_=ps[:]), [psn], [(f"yo{i}", jj, half)])
                    else:
                        P.op("dve", lambda e, ps=ps, i=i, jj=jj, half=half: e.tensor_copy(out=yo[i][:, jj, half * 512:(half + 1) * 512], in_=ps[:]), [psn], [(f"yo{i}", jj, half)])
            P.dma("sp", ys[b * MB:(b + 1) * MB, :].rearrange("(j p) d -> p j d", p=128), yo[i][:], reads=[(f"yo{i}", a, c) for a in range(2) for c in range(2)], writes=[("ys", b)], stream=f"sty{i}")

        P.barrier()
        at = [baseB]
        P.dma("sp", gb[:], g3.partition_broadcast(128), writes=["gb"], stream="m_gb")
        NBUF = 4
        y0 = [sb(f"y0{i}", [128, D], BF16, at) for i in range(NBUF)]
        y1 = [sb(f"y1{i}", [128, D], BF16, at) for i in range(NBUF)]
        xf = [sb(f"xf{i}", [128, D], F32, at) for i in range(NBUF)]
        ot = [sb(f"ot{i}", [128, D], F32, at) for i in range(NBUF)]
        junkf = sb("junkf", [128, D], BF16, at)
        statf = sb("statf", [128, 2 * NBUF], F32, at)

        def cmb_issue(t):
            i = t % NBUF
            P.dma("sp", xf[i][:], x1s[t * 128:(t + 1) * 128, :], reads=[("x1s", t)], writes=[f"xf{i}"], stream=f"ldxf{i}")
            for kk, yb, ybn in ((0, y0, "y0"), (1, y1, "y1")):
                P.op("pool", lambda e, yb=yb, i=i, kk=kk, t=t: e.indirect_dma_start(out=yb[i][:], out_offset=None, in_=ys, in_offset=bass.IndirectOffsetOnAxis(ap=desti[:, kk, t:t + 1], axis=0)),
                     [("ys", bb) for bb in range(NBLK)] + ["desti"], [f"{ybn}{i}"], stream=f"gy{ybn}{i}")

        for t in range(min(NBUF - 1, NA)):
            cmb_issue(t)
        for t in range(NA):
            i = t % NBUF
            s0, s1 = statf[:, 2 * i:2 * i + 1], statf[:, 2 * i + 1:2 * i + 2]
            P.op("dve", lambda e, i=i, t=t: e.scalar_tensor_tensor(out=xf[i][:], in0=y0[i][:], scalar=wk[:, 0, t:t + 1], in1=xf[i][:], op0=ALU.mult, op1=ALU.add), [f"y0{i}", "wk0", f"xf{i}"], [f"xf{i}"])
            P.op("dve", lambda e, i=i, t=t: e.scalar_tensor_tensor(out=xf[i][:], in0=y1[i][:], scalar=wk[:, 1, t:t + 1], in1=xf[i][:], op0=ALU.mult, op1=ALU.add), [f"y1{i}", "wk1", f"xf{i}"], [f"xf{i}"])
            P.op("act", lambda e, i=i, s0=s0: e.activation(out=junkf[:], in_=xf[i][:], func=AF.Square, accum_out=s0), [f"xf{i}"], ["junkf", f"sf0{i}"])
            rstd_ops(s0, s1, [f"sf0{i}"], [f"sf1{i}"])
            P.op("dve", lambda e, i=i, s1=s1: e.scalar_tensor_tensor(out=ot[i][:], in0=xf[i][:], scalar=s1, in1=gb[:], op0=ALU.mult, op1=ALU.mult), [f"xf{i}", f"sf1{i}", "gb"], [f"ot{i}"])
            P.dma("sp", out[t * 128:(t + 1) * 128, :], ot[i][:], reads=[f"ot{i}"], writes=[("out", t)], stream=f"outs{i}")
            if t + NBUF - 1 < NA:
                cmb_issue(t + NBUF - 1)

        P.emit(st, out_streams=[f"outs{i}" for i in range(NBUF)])
    return nc


def make_consts():
    c = np.zeros((128, 6 * 128), np.float32)
    p = np.arange(128)[:, None]
    j = np.arange(128)[None, :]
    c[:, 0:128] = (p == j)
    c[:, 128:256] = (p <= j)
    c[:, 256:384] = (p < j)
    c[:, 384:512] = (p == 127)
    c[:, 512:640] = 1.0
    c[:, 640] = np.arange(128)
    c[:, 641:641 + 96] = 256.0 * np.arange(96)[None, :]
    return c


_CACHE = {}


def run(inputs, n_cores=8, NSEQ=4, debug=False):
    key = (NSEQ, debug)
    if key not in _CACHE:
        _CACHE[key] = build_program(NSEQ, debug)
    nc = _CACHE[key]
    x = np.ascontiguousarray(np.asarray(inputs["x"], dtype=np.float32))
    B = x.shape[0]
    assert B == n_cores * NSEQ
    xs = x.reshape(n_cores, NSEQ * 2048, D)
    shared = {k: np.ascontiguousarray(np.asarray(v, dtype=np.float32)) for k, v in inputs.items() if k != "x"}
    shared["consts"] = make_consts()
    in_maps = []
    for c in range(n_cores):
        m = dict(shared)
        m["x"] = xs[c]
        in_maps.append(m)
    res = run_bass_kernel_spmd(nc, in_maps, core_ids=list(range(n_cores)))
    outs = np.stack([r["out"] for r in res.results], 0).reshape(B, 2048, D)
    if debug:
        return outs, [r for r in res.results]
    return outs


def kernel(**inputs):
    return run(inputs, 8, 4).astype(np.float32)
```

```python
import numpy as np
from contextlib import ExitStack
import concourse.bass as bass
import concourse.mybir as mybir
from concourse.bass_utils import run_bass_kernel_spmd

F32 = mybir.dt.float32
BF16 = mybir.dt.bfloat16
I32 = mybir.dt.int32
ALU = mybir.AluOpType
AF = mybir.ActivationFunctionType
AX = mybir.AxisListType

D = 1024
FILL = False
NH = 16
NE = 32
DE = 512
DIN = 8208
EPS = 1e-6
MB = 256


class Op:
    __slots__ = ("eng", "fn", "reads", "writes", "stream", "idx", "waits", "signal", "ticket", "bar")

    def __init__(self, eng, fn, reads, writes, stream, bar=False):
        self.eng = eng
        self.fn = fn
        self.reads = reads
        self.writes = writes
        self.stream = stream
        self.waits = {}
        self.signal = False
        self.ticket = None
        self.bar = bar


class Prog:
    ENGS = ("pe", "act", "dve", "pool", "sp")

    def __init__(self, nc):
        self.nc = nc
        self.ops = []

    def op(self, eng, fn, reads=(), writes=(), stream=None):
        o = Op(eng, fn, tuple(reads), tuple(writes), stream)
        o.idx = len(self.ops)
        self.ops.append(o)
        return o

    def dma(self, eng, out, in_, reads=(), writes=(), stream="ld", **kw):
        return self.op(eng, lambda e: e.dma_start(out=out, in_=in_, **kw), reads, writes, stream=stream)

    def barrier(self):
        for en in self.ENGS:
            o = Op(en, lambda e: e.nop(), (), (), None, bar=True)
            o.idx = len(self.ops)
            self.ops.append(o)

    def build(self):
        ops = self.ops
        last_w, readers = {}, {}
        last_on_eng = {}
        deps = [None] * len(ops)
        bar_streams = [None] * len(ops)
        run = {}
        run_at = [None] * len(ops)
        for o in ops:
            run_at[o.idx] = dict(run)
            d = set()
            if o.bar:
                for en, li in last_on_eng.items():
                    d.add(li)
                bar_streams[o.idx] = dict(run)
            else:
                for r in o.reads:
                    if r in last_w:
                        d.add(last_w[r])
                for w in o.writes:
                    if w in last_w:
                        d.add(last_w[w])
                    for rd in readers.get(w, ()):
                        d.add(rd)
                for r in o.reads:
                    readers.setdefault(r, []).append(o.idx)
                for w in o.writes:
                    last_w[w] = o.idx
                    readers[w] = []
            d.discard(o.idx)
            deps[o.idx] = d
            if o.stream is not None:
                run[o.stream] = run.get(o.stream, 0) + 1
            else:
                last_on_eng[o.eng] = o.idx
            if o.bar:
                pass
        needed = [dict() for _ in ops]
        for o in ops:
            for p in deps[o.idx]:
                po = ops[p]
                if po.stream is None and po.eng == "pe" and o.eng == "pe" and o.stream is None and not o.bar:
                    continue
                if po.stream is None:
                    po.signal = True
                    dm = ("eng", po.eng)
                else:
                    dm = ("dma", po.stream)
                if dm not in needed[o.idx] or needed[o.idx][dm] < p:
                    needed[o.idx][dm] = p
            if o.bar:
                for s in bar_streams[o.idx]:
                    needed[o.idx][("dma", s)] = -1
        ecount = {}
        for o in ops:
            if o.stream is None and o.signal:
                ecount[o.eng] = ecount.get(o.eng, 0) + 1
                o.ticket = ecount[o.eng]
        waited = {e: {} for e in self.ENGS}
        for o in ops:
            w = {}
            for dm, p in needed[o.idx].items():
                if dm[0] == "dma":
                    val = 16 * run_at[o.idx].get(dm[1], 0)
                else:
                    val = ops[p].ticket
                if val == 0 or waited[o.eng].get(dm, 0) >= val:
                    continue
                waited[o.eng][dm] = val
                w[dm] = val
            o.waits = w
        doms = []
        for o in ops:
            dm = ("dma", o.stream) if o.stream is not None else ("eng", o.eng)
            if dm not in doms and (o.signal or o.stream is not None):
                doms.append(dm)
        self.run_total = run
        return doms

    def emit(self, st, out_streams=()):
        nc = self.nc
        doms = self.build()
        sems = {}
        for dm in doms:
            sems[dm] = st.enter_context(nc.semaphore("s_" + "_".join(str(x) for x in dm)))
        block = st.enter_context(nc.Block())
        ops = self.ops
        run_total = self.run_total

        def make(engname):
            def body(e):
                for o in ops:
                    if o.eng != engname:
                        continue
                    for dm, val in o.waits.items():
                        e.wait_ge(sems[dm], val)
                    ins = o.fn(e)
                    if o.stream is not None:
                        ins.then_inc(sems[("dma", o.stream)], 16)
                    elif o.signal:
                        ins.then_inc(sems[("eng", engname)], 1)
                if engname == "sp":
                    for s in out_streams:
                        e.wait_ge(sems[("dma", s)], 16 * run_total[s])
            return body

        block.tensor(make("pe"))
        block.scalar(make("act"))
        block.vector(make("dve"))
        block.gpsimd(make("pool"))
        block.sync(make("sp"))


def build_program(NSEQ, debug=False):
    nc = bass.Bass("TRN2", target_bir_lowering=False)
    TOK = NSEQ * 2048
    NT = TOK // 128
    NROWS = 2 * TOK + NE * MB
    NBLK = NROWS // MB

    def din(name, shape, dt=F32):
        return nc.dram_tensor(name, list(shape), dt, kind="ExternalInput").ap()

    def dscr(name, shape, dt, out=False):
        return nc.dram_tensor(name, list(shape), dt, kind=("ExternalOutput" if out else "Internal")).ap()

    x = din("x", [TOK, D])
    g1 = din("norm_mix_g", [D]); g2 = din("norm_ffn_g", [D]); g3 = din("norm_final_g", [D])
    w_in = din("w_in", [D, DIN])
    conv_w = din("conv_w", [3, D]); conv_b = din("conv_b", [D]); b_forget = din("b_forget", [NH])
    w_co = din("w_conv_out", [D, D]); w_ao = din("w_att_out", [D, D]); w_o = din("w_out", [D, D])
    w_rg = din("w_router_group", [D, 4]); w_re = din("w_router_expert", [4, D, 8])
    w_eg = din("w_e_gate", [NE, D, DE]); w_eu = din("w_e_up", [NE, D, DE]); w_ed = din("w_e_down", [NE, DE, D])
    consts = din("consts", [128, 6 * 128])
    out = nc.dram_tensor("out", [TOK, D], F32, kind="ExternalOutput").ap()

    win_bf = dscr("win_bf", [128, 8, DIN], BF16)
    wsq_bf = dscr("wsq_bf", [3, 128, 8, D], BF16)
    wall_bf = dscr("wall_bf", [NE * 128, 12288], BF16)
    x1s = dscr("x1s", [TOK, D], F32, out=debug)
    h2s = dscr("h2s", [TOK, D], BF16)
    xs = dscr("xs", [NROWS, D], BF16)
    ys = dscr("ys", [NROWS, D], BF16)

    st = ExitStack()
    with st:
        off = [16544]

        def sb(name, shape, dt, at=None):
            nbytes = int(np.prod(shape[1:])) * (4 if dt in (F32, I32) else 2)
            nbytes = (nbytes + 31) // 32 * 32
            if at is None:
                o = off[0]
                off[0] += nbytes
            else:
                o = at[0]
                at[0] += nbytes
            assert o + nbytes <= 229344, (name, o, nbytes)
            return nc.alloc_sbuf_tensor_at(name, list(shape), dt, offset=o)

        psum = [st.enter_context(nc.psum_tensor(f"ps{i}", [128, 512], F32)) for i in range(6)]
        psT = [st.enter_context(nc.psum_tensor(f"psT{i}", [128, 1024], BF16)) for i in range(2)]

        P = Prog(nc)
        cst = sb("cst", [128, 6 * 128], F32)
        ident_bf = sb("ident_bf", [128, 128], BF16)
        tri_bf = sb("tri_bf", [128, 128], BF16)
        ustr_bf = sb("ustr_bf", [128, 128], BF16)
        ones_bf = sb("ones_bf", [128, 128], BF16)
        tri_f = cst[:, 128:256]
        e127_f = cst[:, 384:512]
        iota_p = cst[:, 640:641]
        thr = cst[:, 641:641 + 96]
        cwT = sb("cwT", [128, 3, 8], F32)
        cbT = sb("cbT", [128, 8], F32)
        bfb = sb("bfb", [128, NH], F32)
        wr_bf = sb("wr_bf", [128, 8, 36], BF16)
        logits = sb("logits", [128, NT, 36], F32)
        gb = sb("gb", [128, D], F32)
        gb2 = sb("gb2", [128, D], F32)
        P.dma("sp", cst[:], consts, writes=["cst"], stream="m_cst")
        P.op("dve", lambda e: e.tensor_copy(out=ident_bf[:], in_=cst[:, 0:128]), ["cst"], ["ident"])
        P.op("dve", lambda e: e.tensor_copy(out=tri_bf[:], in_=cst[:, 128:256]), ["cst"], ["tri"])
        P.op("dve", lambda e: e.tensor_copy(out=ustr_bf[:], in_=cst[:, 256:384]), ["cst"], ["ustr"])
        P.op("dve", lambda e: e.tensor_copy(out=ones_bf[:], in_=cst[:, 512:640]), ["cst"], ["ones"])
        with nc.allow_non_contiguous_dma(reason="tiny per-channel params"):
            P.dma("sp", cwT[:], conv_w.rearrange("t (c p) -> p t c", p=128), writes=["cwT"], allow_slow_non_contiguous=True, stream="m_cw")
            P.dma("sp", cbT[:], conv_b.rearrange("(c p) -> p c", p=128), writes=["cbT"], allow_slow_non_contiguous=True, stream="m_cb")
        P.dma("sp", bfb[:], b_forget.partition_broadcast(128), writes=["bfb"], stream="m_bf")
        P.dma("sp", gb[:], g1.partition_broadcast(128), writes=["gb"], stream="m_gb")
        P.dma("sp", gb2[:], g2.partition_broadcast(128), writes=["gb2"], stream="m_gb2")
        base = off[0]

        at = [base]
        stgA = [sb(f"stg{i}", [128, 8, 512], F32, at) for i in range(4)]
        stbA = [sb(f"stb{i}", [128, 8, 512], BF16, at) for i in range(4)]
        cnt = [0]

        def convert(src_ap, dst_ap, kk, ncol, eng_cast, stq="pool", wres="wscr", stg=None, stb=None, tag="a"):
            stg = stg or stgA
            stb = stb or stbA
            i = cnt[0] % len(stg)
            cnt[0] += 1
            P.dma("sp", stg[i][:, 0:kk, 0:ncol], src_ap, reads=[], writes=[f"stg{tag}{i}"], stream=f"cv{tag}{i}")
            if eng_cast == "act":
                P.op("act", lambda e: e.copy(out=stb[i][:, 0:kk, 0:ncol], in_=stg[i][:, 0:kk, 0:ncol]), [f"stg{tag}{i}"], [f"stb{tag}{i}"])
            else:
                P.op(eng_cast, lambda e: e.tensor_copy(out=stb[i][:, 0:kk, 0:ncol], in_=stg[i][:, 0:kk, 0:ncol]), [f"stg{tag}{i}"], [f"stb{tag}{i}"])
            P.dma(stq, dst_ap, stb[i][:, 0:kk, 0:ncol], reads=[f"stb{tag}{i}"], writes=[wres], stream=f"wst{tag}{i}")

        NZ = NROWS // 256
        engs = ["dve", "act", "dve"]
        ci = 0
        for c0 in range(0, DIN, 512):
            nco = min(512, DIN - c0)
            convert(w_in[:, c0:c0 + nco].rearrange("(k p) n -> p k n", p=128), win_bf[:, :, c0:c0 + nco], 8, nco, engs[ci % 3]); ci += 1
        for mi, wm in enumerate((w_co, w_ao, w_o)):
            for c0 in range(0, D, 512):
                convert(wm[:, c0:c0 + 512].rearrange("(k p) n -> p k n", p=128), wsq_bf[mi, :, :, c0:c0 + 512], 8, 512, engs[ci % 3]); ci += 1
        wr_f = sb("wr_f", [128, 8, 36], F32, at)
        with nc.allow_non_contiguous_dma(reason="tiny router weights"):
            P.dma("sp", wr_f[:, :, 0:4], w_rg.rearrange("(k p) n -> p k n", p=128), writes=["wr_f0"], allow_slow_non_contiguous=True, stream="m_wr")
            for g in range(4):
                P.dma("sp", wr_f[:, :, 4 + 8 * g:12 + 8 * g], w_re[g].rearrange("(k p) n -> p k n", p=128), writes=[f"wr_f{g + 1}"], allow_slow_non_contiguous=True, stream="m_wr")
        P.op("dve", lambda e: e.tensor_copy(out=wr_bf[:], in_=wr_f[:]), [f"wr_f{i}" for i in range(5)], ["wr_bf"])
        P.barrier()

        at = [base]
        wbuf = [sb(f"wbuf{i}", [128, 8, 512], BF16, at) for i in range(3)]
        hT = sb("hT", [128, 8, 2048], BF16, at)
        attT = sb("attT", [128, 8, 2048], BF16, at)
        wf_bf = sb("wf_bf", [128, 8, 16], BF16, at)
        xt = [sb(f"xt{i}", [128, D], F32, at) for i in range(2)]
        hbf = [sb(f"hbf{i}", [128, D], BF16, at) for i in range(2)]
        junk = sb("junk", [128, D], BF16, at)
        stat = sb("stat", [128, 8], F32, at)
        spall = sb("spall", [128, 16, NH], F32, at)
        cumneg = sb("cumneg", [128, 16, NH], F32, at)
        crefB = sb("crefB", [128, 16, NH], F32, at)
        ftmp = sb("ftmp", [128, NH], F32, at)
        ftmp4 = sb("ftmp4", [128, 4, NH], F32, at)
        baseX = at[0]
        vaug = [sb(f"vaug{i}", [128, 16, 2, 128], BF16, at) for i in range(2)]
        qTp = [sb(f"qTp{i}", [128, 2, 2048], BF16, at) for i in range(2)]
        kTp = [sb(f"kTp{i}", [128, 2048], BF16, at) for i in range(2)]
        wqk = [sb(f"wqk{i}", [128, 8, 3, 128], BF16, at) for i in range(2)]
        biasT = [sb(f"biasT{i}", [128, 16, 16], F32, at) for i in range(2)]
        PT = [sb(f"PT{i}", [128, 512], BF16, at) for i in range(6)]
        rden = [sb(f"rden{i}", [128, 512], F32, at) for i in range(2)]
        cvg = [sb(f"cvg{i}", [128, 2, 512], F32, at) for i in range(2)]
        cvb = [sb(f"cvb{i}", [128, 2, 512], BF16, at) for i in range(2)]
        zt = sb("zt", [128, 2048], BF16, at)
        zstate = {"n": 0}
        ZPER = (NZ + NSEQ * 8 - 1) // (NSEQ * 8)

        def zero_fill_step():
            for _ in range(ZPER):
                zi = zstate["n"]
                if zi < NZ:
                    P.dma("sp", xs[zi * 256:(zi + 1) * 256, :].rearrange("(p j) d -> p (j d)", j=2), zt[:], reads=["zt"], writes=[("xsz", zi)], stream="zx")
                    zstate["n"] = zi + 1
        endA12 = at[0]
        cjobs = []
        for ex in range(NE):
            rows = wall_bf[ex * 128:(ex + 1) * 128, :]
            for wsrc, c0 in ((w_eg, 0), (w_eu, 4096)):
                sv = wsrc[ex].rearrange("(k p) n -> p k n", p=128)
                dv = rows[:, c0:c0 + 4096].rearrange("p (k n) -> p k n", k=8)
                for q in range(4):
                    cjobs.append((sv[:, 2 * q:2 * q + 2, :], dv[:, 2 * q:2 * q + 2, :]))
            sv = w_ed[ex].rearrange("(k p) n -> p k n", p=128)
            dv = rows[:, 8192:12288].rearrange("p (k n) -> p k n", k=4)
            for k in range(4):
                cjobs.append((sv[:, k, :].rearrange("p (a n) -> p a n", a=2), dv[:, k, :].rearrange("p (a n) -> p a n", a=2)))
        cst8 = {"next": 0, "pend": None}

        def conv_finish():
            m = cst8["pend"]
            if m is not None:
                i = m % 2
                eng = "dve"
                P.op(eng, lambda e, i=i: e.tensor_copy(out=cvb[i][:], in_=cvg[i][:]), [f"cvg{i}"], [f"cvb{i}"])
                P.dma("sp", cjobs[m][1], cvb[i][:], reads=[f"cvb{i}"], writes=["wex"], stream=f"cvs{i}")
                cst8["pend"] = None

        def conv_step():
            m = cst8["next"]
            old = cst8["pend"]
            if m < len(cjobs):
                i = m % 2
                P.dma("sp", cvg[i][:], cjobs[m][0], writes=[f"cvg{i}"], stream=f"cvl{i}")
            conv_finish()
            if m < len(cjobs):
                cst8["pend"] = m
                cst8["next"] = m + 1
        CONV_PER_PAIR = (len(cjobs) + NSEQ * 8 - 1) // (NSEQ * 8)
        at = [baseX]
        zT = sb("zT", [128, 8, 516], BF16, at)
        cS = [sb(f"cS{i}", [128, 512], F32, at) for i in range(2)]
        acc = [sb(f"acc{i}", [128, 512], F32, at) for i in range(2)]
        cvT = sb("cvT", [128, 8, 512], BF16, at)
        sgc = sb("sgc", [128, 8, 512], BF16, at)
        sga = sb("sga", [128, 8, 512], BF16, at)
        t1 = [sb(f"t1{i}", [128, 512], F32, at) for i in range(2)]
        mT = sb("mT", [128, 8, 512], BF16, at)
        xr = [sb(f"xr{i}", [128, D], F32, at) for i in range(2)]
        x1t = [sb(f"x1t{i}", [128, D], F32, at) for i in range(2)]
        h2t = [sb(f"h2t{i}", [128, D], BF16, at) for i in range(2)]
        h2T = [sb(f"h2T{i}", [128, 8, 128], BF16, at) for i in range(2)]
        junk3 = sb("junk3", [128, D], BF16, at)
        stat3 = sb("stat3", [128, 8], F32, at)
        endA3 = at[0]

        inv_d = 1.0 / D

        def rstd_ops(sumsq_ap, out_ap, rn, wn):
            P.op("dve", lambda e: e.tensor_scalar(out=out_ap, in0=sumsq_ap, scalar1=inv_d, scalar2=EPS, op0=ALU.mult, op1=ALU.add), rn, wn)
            P.op("act", lambda e: e.activation(out=out_ap, in_=out_ap, func=AF.Sqrt), wn, wn)
            P.op("dve", lambda e: e.reciprocal(out=out_ap, in_=out_ap), wn, wn)

        COLS = {"b": 0, "c": 1024, "u": 2048, "q": 3072, "k": 4096, "v": 5120, "f": 6144, "gc": 6160, "ga": 7184}
        wcount = [0]

        wplan = []
        for _ch in range(4):
            for half in range(2):
                for nm in ("c", "u", "b"):
                    wplan.append(win_bf[:, :, COLS[nm] + half * 512:COLS[nm] + (half + 1) * 512])
            for nm in ("gc", "ga"):
                for half in range(2):
                    wplan.append(win_bf[:, :, COLS[nm] + half * 512:COLS[nm] + (half + 1) * 512])
            for half in range(2):
                wplan.append(wsq_bf[0, :, :, half * 512:(half + 1) * 512])
                wplan.append(wsq_bf[1, :, :, half * 512:(half + 1) * 512])
            wplan.append(wsq_bf[2, :, :, 0:512])
            wplan.append(wsq_bf[2, :, :, 512:1024])
        wstate = {"cur": 0, "issued": 0}

        def w_issue(g):
            i = g % 3
            P.dma("sp", wbuf[i][:], wplan[g], reads=["wscr"], writes=[f"wbuf{i}"], stream=f"ldw{i}")

        def w_reset():
            wstate["cur"] = 0
            wstate["issued"] = 0
            for g in range(3):
                w_issue(g)
            wstate["issued"] = 3

        def load_w(src_ap=None):
            g = wstate["cur"]
            wstate["cur"] += 1
            return wbuf[g % 3], f"wbuf{g % 3}"

        def w_done(n=1):
            for _ in range(n):
                g = wstate["issued"]
                if g < len(wplan):
                    w_issue(g)
                    wstate["issued"] += 1

        pcount = [0]

        def next_ps():
            i = pcount[0] % 6
            pcount[0] += 1
            return psum[i], f"ps{i}"

        P.dma("sp", wf_bf[:], win_bf[:, :, COLS["f"]:COLS["f"] + 16], reads=["wscr"], writes=["wf_bf"], stream="ldwf")


        def a1_tiles(s, tiles):
            t0 = s * 2048
            tiles = list(tiles)

            def stA(tt):
                i = tt % 2
                s0, s1 = stat[:, 2 * i:2 * i + 1], stat[:, 2 * i + 1:2 * i + 2]
                P.dma("sp", xt[i][:], x[t0 + tt * 128:t0 + (tt + 1) * 128, :], writes=[f"xt{i}"], stream=f"ldxt{i}")
                P.op("act", lambda e, i=i, s0=s0: e.activation(out=junk[:], in_=xt[i][:], func=AF.Square, accum_out=s0),
                     [f"xt{i}"], ["junk", f"stat0{i}"])
                rstd_ops(s0, s1, [f"stat0{i}"], [f"stat1{i}"])
                P.op("dve", lambda e, i=i, s1=s1: e.scalar_tensor_tensor(out=hbf[i][:], in0=xt[i][:], scalar=s1, in1=gb[:], op0=ALU.mult, op1=ALU.mult),
                     [f"xt{i}", f"stat1{i}", "gb"], [f"hbf{i}"])

            def stB(tt):
                i = tt % 2
                pt, ptn = psT[tt % 2], f"psT{tt % 2}"
                def tr(e, i=i, pt=pt):
                    for k in range(8):
                        ins = e.transpose(out=pt[:, k * 128:(k + 1) * 128], in_=hbf[i][:, k * 128:(k + 1) * 128], identity=ident_bf[:])
                    return ins
                P.op("pe", tr, [f"hbf{i}", "ident"], [ptn])
                P.op("act", lambda e, pt=pt, tt=tt: e.copy(out=hT[:, :, tt * 128:(tt + 1) * 128], in_=pt[:].rearrange("p (k t) -> p k t", k=8)),
                     [ptn], [("hT", tt)])

            def stC(group):
                ps, psn = next_ps()
                def fl(e, ps=ps, group=group):
                    for gi, tt in enumerate(group):
                        for k in range(8):
                            ins = e.matmul(ps[:, gi * 16:(gi + 1) * 16], lhsT=hT[:, k, tt * 128:(tt + 1) * 128], rhs=wf_bf[:, k, :], start=(k == 0), stop=(k == 7))
                    return ins
                P.op("pe", fl, [("hT", tt) for tt in group] + ["wf_bf"], [psn])
                n = len(group)
                g0 = group[0]
                fv = ftmp4[:, 0:n, :]
                P.op("dve", lambda e, ps=ps, n=n, fv=fv: e.tensor_tensor(out=fv, in0=ps[:, 0:16 * n].rearrange("p (a b) -> p a b", a=n), in1=bfb[:].unsqueeze(1).to_broadcast([128, n, NH]), op=ALU.add), [psn, "bfb"], ["ftmp"])
                P.op("act", lambda e, fv=fv: e.activation(out=fv, in_=fv, func=AF.Exp, scale=-1.0), ["ftmp"], ["ftmp"])
                P.op("act", lambda e, fv=fv, g0=g0, n=n: e.activation(out=spall[:, g0:g0 + n, :], in_=fv, func=AF.Ln, bias=1.0), ["ftmp"], [("sp", tt) for tt in group])

            n = len(tiles)
            for idx in range(n + 1):
                if idx < n:
                    stA(tiles[idx])
                if idx >= 1:
                    stB(tiles[idx - 1])
            for g in range(0, n, 4):
                stC(tiles[g:g + 4])

        def a1_finish(s):
            for tt in range(16):
                ps, psn = next_ps()
                def cm(e, ps=ps, tt=tt):
                    ins = e.matmul(ps[:, 0:16], lhsT=tri_f, rhs=spall[:, tt, :], start=True, stop=(tt == 0))
                    if tt > 0:
                        ins = e.matmul(ps[:, 0:16], lhsT=e127_f, rhs=cumneg[:, tt - 1, :], start=False, stop=True)
                    return ins
                P.op("pe", cm, [("sp", tt), "cst"] + ([("cum", tt - 1)] if tt else []), [psn])
                P.op("dve", lambda e, ps=ps, tt=tt: e.tensor_copy(out=cumneg[:, tt, :], in_=ps[:, 0:16]), [psn], [("cum", tt)])
            ps, psn = next_ps()
            P.op("pe", lambda e, ps=ps: e.matmul(ps[:, 0:256], lhsT=e127_f, rhs=cumneg[:].rearrange("p a b -> p (a b)"), start=True, stop=True),
                 [("cum", tt) for tt in range(16)] + ["cst"], [psn])
            P.op("dve", lambda e, ps=ps: e.tensor_copy(out=crefB[:].rearrange("p a b -> p (a b)"), in_=ps[:, 0:256]), [psn], ["crefB"])


        a1_tiles(0, range(16))
        a1_finish(0)
        for s in range(NSEQ):
            t0 = s * 2048
            for j in range(8):
                pb = j % 2
                if j == 0:
                    P.op("pool", lambda e: e.memset(zt[:], 0.0), [], ["zt"])
                zero_fill_step()
                for wi, nm in enumerate(("q", "k", "v")):
                    P.dma("sp", wqk[pb][:, :, wi, :], win_bf[:, :, COLS[nm] + j * 128:COLS[nm] + (j + 1) * 128], reads=["wscr"], writes=[f"wqk{pb}_{wi}"], stream=f"ldq{pb}_{wi}")
                if j < 2:
                    P.op("pool", lambda e, pb=pb: e.memset(qTp[pb][:, 0, :], 0.0), [], [(f"qTp{pb}", c) for c in range(4)])
                    P.op("pool", lambda e, pb=pb: e.memset(qTp[pb][:, 1, :], 0.0), [], [(f"qTp{pb}", c) for c in range(4)])
                for wi, dst, dn in ((0, qTp[pb], f"qTp{pb}"), (1, kTp[pb], f"kTp{pb}")):
                    for ch in range(4):
                        ps, psn = next_ps()
                        def pj(e, ps=ps, ch=ch, wi=wi, pb=pb):
                            for k in range(8):
                                ins = e.matmul(ps[:], lhsT=wqk[pb][:, k, wi, :], rhs=hT[:, k, ch * 512:(ch + 1) * 512], start=(k == 0), stop=(k == 7))
                            return ins
                        P.op("pe", pj, [f"wqk{pb}_{wi}"] + [("hT", tt) for tt in range(ch * 4, ch * 4 + 4)], [psn])
                        if wi == 0:
                            P.op("act", lambda e, ps=ps, dst=dst, ch=ch: e.copy(out=dst[0:64, 0, ch * 512:(ch + 1) * 512], in_=ps[0:64, :]), [psn], [(dn, ch)])
                            P.op("dve", lambda e, ps=ps, dst=dst, ch=ch: e.tensor_copy(out=dst[64:128, 1, ch * 512:(ch + 1) * 512], in_=ps[64:128, :]), [psn], [(dn, ch)])
                        elif ch % 2 == 0:
                            P.op("act", lambda e, ps=ps, dst=dst, ch=ch: e.copy(out=dst[:, ch * 512:(ch + 1) * 512], in_=ps[:]), [psn], [(dn, ch)])
                        else:
                            P.op("dve", lambda e, ps=ps, dst=dst, ch=ch: e.tensor_copy(out=dst[:, ch * 512:(ch + 1) * 512], in_=ps[:]), [psn], [(dn, ch)])
                if j < 2:
                    P.op("pool", lambda e, pb=pb: e.memset(vaug[pb][:].rearrange("p a b c -> p (a b c)"), 1.0), [], [(f"vaug{pb}", g) for g in range(4)])
                for g4 in range(4):
                    ps, psn = next_ps()
                    def pv(e, ps=ps, g4=g4, pb=pb):
                        for q4 in range(4):
                            tt = g4 * 4 + q4
                            for k in range(8):
                                ins = e.matmul(ps[:, q4 * 128:(q4 + 1) * 128], lhsT=hT[:, k, tt * 128:(tt + 1) * 128], rhs=wqk[pb][:, k, 2, :], start=(k == 0), stop=(k == 7))
                        return ins
                    P.op("pe", pv, [f"wqk{pb}_2"] + [("hT", tt) for tt in range(g4 * 4, g4 * 4 + 4)], [psn])
                    psv = ps[:].rearrange("p (t h d) -> p t h d", t=4, h=2)
                    P.op("dve", lambda e, psv=psv, g4=g4, pb=pb: e.tensor_copy(out=vaug[pb][:, g4 * 4:(g4 + 1) * 4, 0, 0:64], in_=psv[:, :, 0, :]), [psn], [(f"vaug{pb}", g4)])
                    P.op("dve", lambda e, psv=psv, g4=g4, pb=pb: e.tensor_copy(out=vaug[pb][:, g4 * 4:(g4 + 1) * 4, 1, 64:128], in_=psv[:, :, 1, :]), [psn], [(f"vaug{pb}", g4)])
                groups = []
                for hh in range(2):
                    h = 2 * j + hh
                    bi = hh
                    def bt(e, bi=bi, h=h):
                        for qb in range(16):
                            ins = e.tensor_scalar(out=biasT[bi][:, 0:qb + 1, qb], in0=cumneg[:, 0:qb + 1, h], scalar1=crefB[:, 2 * (qb // 2), h:h + 1], scalar2=None, op0=ALU.subtract)
                        return ins
                    P.op("pool", bt, [("cum", tt) for tt in range(16)] + ["crefB"], [f"biasT{bi}"])
                    for qc in range(4):
                        for kb in range(4 * qc + 4):
                            groups.append((hh, qc, kb))
                LA = 4
                SB5 = [(psum[3], "ps3"), (psum[4], "ps4"), (psum[5], "ps5"), (psT[0][:].bitcast(F32), "psT0"), (psT[1][:].bitcast(F32), "psT1")]

                def emit_qk(t, pb=pb):
                    hh, qc, kb = groups[t]
                    lo, hi = 64 * hh, 64 * hh + 64
                    off = 128 * max(0, kb - 4 * qc)
                    pss, pssn = SB5[t % 5]
                    P.op("pe", lambda e: e.matmul(pss[:, off:512], lhsT=kTp[pb][:, kb * 128:(kb + 1) * 128],
                                                  rhs=qTp[pb][:, hh, qc * 512 + off:(qc + 1) * 512], start=True, stop=True),
                         [(f"qTp{pb}", qc), (f"kTp{pb}", kb // 4)], [pssn])

                def emit_rest(t, pb=pb, j=j):
                    hh, qc, kb = groups[t]
                    bi = hh
                    jj = max(0, kb - 4 * qc)
                    off = 128 * jj
                    pss, pssn = SB5[t % 5]
                    oi = (hh * 4 + qc) % 3
                    pso, pson = psum[oi], f"ps{oi}"
                    ptb, ptn2 = PT[t % 6], f"PT{t % 6}"
                    def ex(e):
                        for sp in range(2):
                            c_lo = max(off, 256 * sp)
                            c_hi = 256 * sp + 256
                            if c_lo >= c_hi:
                                continue
                            qb = 4 * qc + 2 * sp
                            ins = e.activation(out=ptb[:, c_lo:c_hi], in_=pss[:, c_lo:c_hi], func=AF.Exp,
                                               bias=biasT[bi][:, kb, qb + 1:qb + 2] if kb > qb else biasT[bi][:, kb, qb:qb + 1], scale=0.125)
                        return ins
                    P.op("act", ex, [pssn, f"biasT{bi}"], [ptn2])
                    if kb >= 4 * qc:
                        P.op("pool", lambda e: e.tensor_tensor(out=ptb[:, off:off + 128], in0=ptb[:, off:off + 128], in1=tri_bf[:], op=ALU.mult),
                             [ptn2, "tri"], [ptn2])
                    last = (kb == 4 * qc + 3)
                    P.op("pe", lambda e: e.matmul(pso[:, off:512], lhsT=vaug[pb][:, kb, hh, :], rhs=ptb[:, off:512], start=(kb == 0), stop=last),
                         [ptn2, (f"vaug{pb}", kb // 4)], [pson])
                    if FILL:
                        fps = psT[1][:].bitcast(F32)
                        P.op("pe", lambda e: e.matmul(fps[:, 0:512], lhsT=ident_bf[:], rhs=hT[:, 0, 0:512], start=True, stop=True), [], ["psT1"])
                    if last:
                        ri = qc % 2
                        cs = slice(qc * 512, (qc + 1) * 512)
                        if hh == 0:
                            P.op("dve", lambda e: e.reciprocal(out=rden[ri][0:64, :], in_=pso[64:128, :]), [pson], [f"rden{ri}"])
                            P.op("dve", lambda e: e.tensor_tensor(out=attT[0:64, j, cs], in0=pso[0:64, :], in1=rden[ri][0:64, :], op=ALU.mult),
                                 [pson, f"rden{ri}"], [("attT", j, qc, 0)])
                        else:
                            P.op("dve", lambda e: e.reciprocal(out=rden[ri][64:128, :], in_=pso[0:64, :]), [pson], [f"rden{ri}"])
                            P.op("dve", lambda e: e.tensor_tensor(out=attT[64:128, j, cs], in0=pso[64:128, :], in1=rden[ri][64:128, :], op=ALU.mult),
                                 [pson, f"rden{ri}"], [("attT", j, qc, 1)])

                cdone = 0
                for t in range(len(groups) + LA):
                    if t < len(groups):
                        emit_qk(t)
                    if t >= LA:
                        emit_rest(t - LA)
                    if t % 6 == 5 and cdone < CONV_PER_PAIR:
                        conv_step()
                        cdone += 1
                while cdone < CONV_PER_PAIR:
                    conv_step()
                    cdone += 1
                if j == 7:
                    conv_finish()
            w_reset()
            P.barrier()

            P.op("pool", lambda e: e.memset(zT[:, :, 0:2], 0.0), [], ["zT"])
            for ch in range(4):
                c0 = ch * 512
                hTr = [("hT", tt) for tt in range(ch * 4, ch * 4 + 4)]
                for half in range(2):
                    wbs = {}
                    for fi in range(4):
                        fc = half * 4 + fi
                    wb, wbn = load_w(win_bf[:, :, COLS["c"] + half * 512:COLS["c"] + (half + 1) * 512])
                    cS_list = []
                    for fi in range(4):
                        fc = half * 4 + fi
                        ps, psn = next_ps()
                        def mm(e, ps=ps, wb=wb, fi=fi, c0=c0):
                            for k in range(8):
                                ins = e.matmul(ps[:], lhsT=wb[:, k, fi * 128:(fi + 1) * 128], rhs=hT[:, k, c0:c0 + 512], start=(k == 0), stop=(k == 7))
                            return ins
                        P.op("pe", mm, [wbn] + hTr, [psn])
                        P.op("act", lambda e, ps=ps, fc=fc: e.copy(out=mT[:, fc, :], in_=ps[:]), [psn], [("mTc", fc), ("mT", fc)])
                    w_done()
                    wb, wbn = load_w(win_bf[:, :, COLS["u"] + half * 512:COLS["u"] + (half + 1) * 512])
                    for fi in range(4):
                        fc = half * 4 + fi
                        ps, psn = next_ps()
                        def mm(e, ps=ps, wb=wb, fi=fi, c0=c0):
                            for k in range(8):
                                ins = e.matmul(ps[:], lhsT=wb[:, k, fi * 128:(fi + 1) * 128], rhs=hT[:, k, c0:c0 + 512], start=(k == 0), stop=(k == 7))
                            return ins
                        P.op("pe", mm, [wbn] + hTr, [psn])
                        P.op("dve", lambda e, ps=ps, fc=fc: e.tensor_tensor(out=zT[:, fc, 2:514], in0=ps[:], in1=mT[:, fc, :], op=ALU.mult), [psn, ("mTc", fc), "zT"], [("zT", fc)])
                    w_done()
                    wb, wbn = load_w(win_bf[:, :, COLS["b"] + half * 512:COLS["b"] + (half + 1) * 512])
                    for fi in range(4):
                        fc = half * 4 + fi
                        ps, psn = next_ps()
                        def mm(e, ps=ps, wb=wb, fi=fi, c0=c0):
                            for k in range(8):
                                ins = e.matmul(ps[:], lhsT=wb[:, k, fi * 128:(fi + 1) * 128], rhs=hT[:, k, c0:c0 + 512], start=(k == 0), stop=(k == 7))
                            return ins
                        P.op("pe", mm, [wbn] + hTr, [psn])
                        ai = fc % 2
                        P.op("pool", lambda e, fc=fc, ai=ai: e.tensor_scalar(out=acc[ai][:], in0=zT[:, fc, 2:514], scalar1=cwT[:, 2, fc:fc + 1], scalar2=cbT[:, fc:fc + 1], op0=ALU.mult, op1=ALU.add),
                             [("zT", fc), "cwT", "cbT"], [f"acc{ai}"])
                        P.op("dve", lambda e, fc=fc, ai=ai: e.scalar_tensor_tensor(out=acc[ai][:], in0=zT[:, fc, 1:513], scalar=cwT[:, 1, fc:fc + 1], in1=acc[ai][:], op0=ALU.mult, op1=ALU.add),
                             [("zT", fc), f"acc{ai}"], [f"acc{ai}"])
                        P.op("dve", lambda e, fc=fc, ai=ai: e.scalar_tensor_tensor(out=acc[ai][:], in0=zT[:, fc, 0:512], scalar=cwT[:, 0, fc:fc + 1], in1=acc[ai][:], op0=ALU.mult, op1=ALU.add),
                             [("zT", fc), f"acc{ai}"], [f"acc{ai}"])
                        P.op("dve", lambda e, ps=ps, fc=fc, ai=ai: e.tensor_tensor(out=cvT[:, fc, :], in0=ps[:], in1=acc[ai][:], op=ALU.mult), [psn, f"acc{ai}"], [("cvT", fc)])
                        P.op("pool", lambda e, fc=fc: e.tensor_copy(out=zT[:, fc, 0:2], in_=zT[:, fc, 512:514]), [("zT", fc), f"acc{ai}"], [("zT", fc)])
                    w_done()
                for (nm, dstg, dgn) in (("gc", sgc, "sgc"), ("ga", sga, "sga")):
                    for half in range(2):
                        wb, wbn = load_w(win_bf[:, :, COLS[nm] + half * 512:COLS[nm] + (half + 1) * 512])
                        for fi in range(4):
                            fc = half * 4 + fi
                            ps, psn = next_ps()
                            def mm(e, ps=ps, wb=wb, fi=fi, c0=c0):
                                for k in range(8):
                                    ins = e.matmul(ps[:], lhsT=wb[:, k, fi * 128:(fi + 1) * 128], rhs=hT[:, k, c0:c0 + 512], start=(k == 0), stop=(k == 7))
                                return ins
                            P.op("pe", mm, [wbn] + hTr, [psn])
                            P.op("act", lambda e, ps=ps, dstg=dstg, fc=fc: e.activation(out=dstg[:, fc, :], in_=ps[:], func=AF.Sigmoid), [psn], [(dgn, fc)])
                        w_done()
                for half in range(2):
                    wbc, wbcn = load_w(wsq_bf[0, :, :, half * 512:(half + 1) * 512])
                    wba, wban = load_w(wsq_bf[1, :, :, half * 512:(half + 1) * 512])
                    for fi in range(4):
                        fo = half * 4 + fi
                        ps1, ps1n = next_ps()
                        def mm1(e, ps=ps1, wb=wbc, fi=fi):
                            for k in range(8):
                                ins = e.matmul(ps[:], lhsT=wb[:, k, fi * 128:(fi + 1) * 128], rhs=cvT[:, k, :], start=(k == 0), stop=(k == 7))
                            return ins
                        P.op("pe", mm1, [wbcn] + [("cvT", k) for k in range(8)], [ps1n])
                        ps2, ps2n = next_ps()
                        def mm2(e, ps=ps2, wb=wba, fi=fi, c0=c0):
                            for k in range(8):
                                ins = e.matmul(ps[:], lhsT=wb[:, k, fi * 128:(fi + 1) * 128], rhs=attT[:, k, c0:c0 + 512], start=(k == 0), stop=(k == 7))
                            return ins
                        P.op("pe", mm2, [wban] + [("attT", k, ch, hh) for k in range(8) for hh in range(2)], [ps2n])
                        ti = fo % 2
                        P.op("dve", lambda e, ps=ps1, fo=fo, ti=ti: e.tensor_tensor(out=t1[ti][:], in0=ps[:], in1=sgc[:, fo, :], op=ALU.mult), [ps1n, ("sgc", fo)], [f"t1{ti}"])
                        P.op("dve", lambda e, ps=ps2, fo=fo, ti=ti: e.tensor_tensor(out=acc[ti][:], in0=ps[:], in1=sga[:, fo, :], op=ALU.mult), [ps2n, ("sga", fo)], [f"acc{ti}"])
                        P.op("pool", lambda e, fo=fo, ti=ti: e.tensor_tensor(out=mT[:, fo, :], in0=t1[ti][:], in1=acc[ti][:], op=ALU.add), [f"t1{ti}", f"acc{ti}", ("mTc", fo)], [("mT", fo), ("mTc", fo)])
                    w_done(2)
                wo0, wo0n = load_w(wsq_bf[2, :, :, 0:512])
                wo1, wo1n = load_w(wsq_bf[2, :, :, 512:1024])
                def stX(q4):
                    tt = ch * 4 + q4
                    gt = s * 16 + tt
                    i = tt % 2
                    P.dma("act", xr[i][:], x[t0 + tt * 128:t0 + (tt + 1) * 128, :], writes=[f"xr{i}"], stream=f"ldxr{i}")
                    for half, (wo, won) in enumerate(((wo0, wo0n), (wo1, wo1n))):
                        ps, psn = next_ps()
                        def mm(e, ps=ps, wo=wo, q4=q4):
                            for k in range(8):
                                ins = e.matmul(ps[:], lhsT=mT[:, k, q4 * 128:(q4 + 1) * 128], rhs=wo[:, k, :], start=(k == 0), stop=(k == 7))
                            return ins
                        P.op("pe", mm, [won] + [("mT", k) for k in range(8)], [psn])
                        P.op("dve", lambda e, ps=ps, i=i, half=half: e.tensor_tensor(out=x1t[i][:, half * 512:(half + 1) * 512], in0=ps[:], in1=xr[i][:, half * 512:(half + 1) * 512], op=ALU.add),
                             [psn, f"xr{i}"], [(f"x1t{i}", half)])
                    P.dma("pool", x1s[t0 + tt * 128:t0 + (tt + 1) * 128, :], x1t[i][:], reads=[(f"x1t{i}", 0), (f"x1t{i}", 1)], writes=[("x1s", gt)], stream=f"stx{i}")

                def stY(q4):
                    tt = ch * 4 + q4
                    gt = s * 16 + tt
                    i = tt % 2
                    s0, s1 = stat3[:, 2 * i:2 * i + 1], stat3[:, 2 * i + 1:2 * i + 2]
                    P.op("act", lambda e, i=i, s0=s0: e.activation(out=junk3[:], in_=x1t[i][:], func=AF.Square, accum_out=s0),
                         [(f"x1t{i}", 0), (f"x1t{i}", 1)], ["junk3", f"s30{i}"])
                    rstd_ops(s0, s1, [f"s30{i}"], [f"s31{i}"])
                    P.op("dve", lambda e, i=i, s1=s1: e.scalar_tensor_tensor(out=h2t[i][:], in0=x1t[i][:], scalar=s1, in1=gb2[:], op0=ALU.mult, op1=ALU.mult),
                         [(f"x1t{i}", 0), (f"x1t{i}", 1), f"s31{i}", "gb2"], [f"h2t{i}"])
                    P.dma("pool", h2s[t0 + tt * 128:t0 + (tt + 1) * 128, :], h2t[i][:], reads=[f"h2t{i}"], writes=[("h2s", gt)], stream=f"sth{i}")

                def stZ(q4):
                    tt = ch * 4 + q4
                    gt = s * 16 + tt
                    i = tt % 2
                    pt, ptn = psT[tt % 2], f"psT{tt % 2}"
                    def tr(e, i=i, pt=pt):
                        for k in range(8):
                            ins = e.transpose(out=pt[:, k * 128:(k + 1) * 128], in_=h2t[i][:, k * 128:(k + 1) * 128], identity=ident_bf[:])
                        return ins
                    P.op("pe", tr, [f"h2t{i}", "ident"], [ptn])
                    P.op("act", lambda e, pt=pt, i=i: e.copy(out=h2T[i][:], in_=pt[:].rearrange("p (k t) -> p k t", k=8)), [ptn], [f"h2T{i}"])
                    ps, psn = next_ps()
                    def rl(e, ps=ps, i=i):
                        for k in range(8):
                            ins = e.matmul(ps[:, 0:36], lhsT=h2T[i][:, k, :], rhs=wr_bf[:, k, :], start=(k == 0), stop=(k == 7))
                        return ins
                    P.op("pe", rl, [f"h2T{i}", "wr_bf"], [psn])
                    P.op("dve", lambda e, ps=ps, gt=gt: e.tensor_copy(out=logits[:, gt, :], in_=ps[:, 0:36]), [psn], ["logits"])

                stX(0)
                stX(1)
                stY(0)
                stY(1)
                stZ(0)
                stX(2)
                stZ(1)
                stY(2)
                stX(3)
                stZ(2)
                stY(3)
                stZ(3)
                w_done(2)
                if s + 1 < NSEQ:
                    a1_tiles(s + 1, range(ch * 4, ch * 4 + 4))
            if s + 1 < NSEQ:
                a1_finish(s + 1)
            P.barrier()

        at = [base]
        NA = NT
        lg = logits[:, :, 0:4]
        le = logits[:, :, 4:36].rearrange("p t (g e) -> p t g e", g=4)
        wk = sb("wk", [128, 2, NA], F32, at)
        desti = sb("desti", [128, 2, NA], I32, at)
        idxi = sb("idxi", [128, NBLK], I32, at)
        baseB = at[0]
        gmax = sb("gmax", [128, NA], F32, at)
        ohg = sb("ohg", [128, NA, 4], F32, at)
        eg = sb("eg", [128, NA, 4], F32, at)
        gsum = sb("gsum", [128, NA], F32, at)
        gval = sb("gval", [128, NA], F32, at)
        lsel = sb("lsel", [128, NA, 8], F32, at)
        ltmp = sb("ltmp", [128, NA, 8], F32, at)
        m1 = sb("m1", [128, NA], F32, at)
        m2 = sb("m2", [128, NA], F32, at)
        oh1 = sb("oh1", [128, NA, 8], F32, at)
        oh2 = sb("oh2", [128, NA, 8], F32, at)
        M1 = sb("M1", [128, NA, 4, 8], F32, at)
        M2 = sb("M2", [128, NA, 4, 8], F32, at)
        Mb = sb("Mb", [128, NA, 32], BF16, at)
        within = sb("within", [128, NA, 32], F32, at)
        tsum = sb("tsum", [128, NA, 32], F32, at)
        carry = sb("carry", [128, NA + 1, 32], F32, at)
        prod = sb("prod", [128, NA, 32], F32, at)
        rk = sb("rk", [128, 2, NA], F32, at)
        nb = sb("nb", [128, 32], F32, at)
        cmpb = sb("cmpb", [128, 32, 32], F32, at)
        endp = sb("endp", [128, 32], F32, at)
        startp = sb("startp", [128, 32], F32, at)
        destf = sb("destf", [128, 2, NA], F32, at)
        cmp2 = sb("cmp2", [128, NBLK, 32], F32, at)
        bef = sb("bef", [128, NBLK], F32, at)

        R = ["logits"]
        bc = lambda ap, shape: ap.to_broadcast(shape)
        P.op("dve", lambda e: e.tensor_reduce(out=gmax[:], in_=lg, axis=AX.X, op=ALU.max), R, ["gmax"])
        P.op("dve", lambda e: e.tensor_tensor(out=eg[:], in0=lg, in1=gmax[:].unsqueeze(2).to_broadcast([128, NA, 4]), op=ALU.subtract), R + ["gmax"], ["eg"])
        P.op("dve", lambda e: e.tensor_single_scalar(out=ohg[:], in_=eg[:], scalar=0.0, op=ALU.is_ge), ["eg"], ["ohg"])
        P.op("act", lambda e: e.activation(out=eg[:], in_=eg[:], func=AF.Exp), ["eg", "ohg"], ["eg"])
        P.op("dve", lambda e: e.tensor_reduce(out=gsum[:], in_=eg[:], axis=AX.X, op=ALU.add), ["eg"], ["gsum"])
        P.op("dve", lambda e: e.reciprocal(out=gval[:], in_=gsum[:]), ["gsum"], ["gval"])
        for g in range(4):
            if g == 0:
                P.op("dve", lambda e: e.tensor_tensor(out=lsel[:], in0=le[:, :, 0, :], in1=ohg[:, :, 0:1].to_broadcast([128, NA, 8]), op=ALU.mult), R + ["ohg"], ["lsel"])
            else:
                P.op("dve", lambda e, g=g: e.tensor_tensor(out=ltmp[:], in0=le[:, :, g, :], in1=ohg[:, :, g:g + 1].to_broadcast([128, NA, 8]), op=ALU.mult), R + ["ohg", "lsel"], ["ltmp"])
                P.op("dve", lambda e: e.tensor_tensor(out=lsel[:], in0=lsel[:], in1=ltmp[:], op=ALU.add), ["ltmp", "lsel"], ["lsel"])
        P.op("dve", lambda e: e.tensor_reduce(out=m1[:], in_=lsel[:], axis=AX.X, op=ALU.max), ["lsel"], ["m1"])
        P.op("dve", lambda e: e.tensor_tensor(out=oh1[:], in0=lsel[:], in1=m1[:].unsqueeze(2).to_broadcast([128, NA, 8]), op=ALU.is_ge), ["lsel", "m1"], ["oh1"])
        P.op("dve", lambda e: e.scalar_tensor_tensor(out=ltmp[:], in0=oh1[:], scalar=-1e9, in1=lsel[:], op0=ALU.mult, op1=ALU.add), ["oh1", "lsel"], ["ltmp"])
        P.op("dve", lambda e: e.tensor_reduce(out=m2[:], in_=ltmp[:], axis=AX.X, op=ALU.max), ["ltmp"], ["m2"])
        P.op("dve", lambda e: e.tensor_tensor(out=oh2[:], in0=ltmp[:], in1=m2[:].unsqueeze(2).to_broadcast([128, NA, 8]), op=ALU.is_ge), ["ltmp", "m2"], ["oh2"])
        P.op("dve", lambda e: e.tensor_tensor(out=m2[:], in0=m2[:], in1=m1[:], op=ALU.subtract), ["m2", "m1", "oh2"], ["m2"])
        P.op("act", lambda e: e.activation(out=m2[:], in_=m2[:], func=AF.Exp), ["m2"], ["m2"])
        P.op("dve", lambda e: e.tensor_scalar(out=m2[:], in0=m2[:], scalar1=1.0, scalar2=None, op0=ALU.add), ["m2"], ["m2"])
        P.op("dve", lambda e: e.reciprocal(out=m2[:], in_=m2[:]), ["m2"], ["m2"])
        P.op("dve", lambda e: e.tensor_tensor(out=wk[:, 0, :], in0=gval[:], in1=m2[:], op=ALU.mult), ["m2", "gval"], ["wk0"])
        P.op("dve", lambda e: e.tensor_tensor(out=wk[:, 1, :], in0=gval[:], in1=wk[:, 0, :], op=ALU.subtract), ["wk0", "gval"], ["wk1"])
        for g in range(4):
            P.op("dve", lambda e, g=g: e.tensor_tensor(out=M1[:, :, g, :], in0=oh1[:], in1=ohg[:, :, g:g + 1].to_broadcast([128, NA, 8]), op=ALU.mult), ["oh1", "ohg"], ["M1"])
            P.op("dve", lambda e, g=g: e.tensor_tensor(out=M2[:, :, g, :], in0=oh2[:], in1=ohg[:, :, g:g + 1].to_broadcast([128, NA, 8]), op=ALU.mult), ["oh2", "ohg"], ["M2"])
        M1f = M1[:].rearrange("p t g e -> p t (g e)")
        M2f = M2[:].rearrange("p t g e -> p t (g e)")
        P.op("dve", lambda e: e.tensor_tensor(out=Mb[:], in0=M1f, in1=M2f, op=ALU.add), ["M1", "M2"], ["Mb"])
        for t in range(NA):
            ps, psn = next_ps()
            def rm(e, ps=ps, t=t):
                e.matmul(ps[:, 0:32], lhsT=ustr_bf[:], rhs=Mb[:, t, :], start=True, stop=True)
                return e.matmul(ps[:, 32:64], lhsT=ones_bf[:], rhs=Mb[:, t, :], start=True, stop=True)
            P.op("pe", rm, ["Mb", "ustr", "ones"], [psn])
            P.op("dve", lambda e, ps=ps, t=t: e.tensor_copy(out=within[:, t, :], in_=ps[:, 0:32]), [psn], ["within"])
            P.op("act", lambda e, ps=ps, t=t: e.copy(out=tsum[:, t, :], in_=ps[:, 32:64]), [psn], ["tsum"])
        P.op("pool", lambda e: e.memset(carry[:, 0, :], 0.0), [], ["carry"])
        def cch(e):
            for t in range(NA):
                ins = e.tensor_tensor(out=carry[:, t + 1, :], in0=carry[:, t, :], in1=tsum[:, t, :], op=ALU.add)
            return ins
        for t in range(NA):
            P.op("pool", lambda e, t=t: e.tensor_tensor(out=carry[:, t + 1, :], in0=carry[:, t, :], in1=tsum[:, t, :], op=ALU.add), ["tsum", "carry"], ["carry"])
        P.op("dve", lambda e: e.tensor_tensor(out=within[:], in0=within[:], in1=carry[:, 0:NA, :], op=ALU.add), ["within", "carry"], ["within"])
        for kk, Mf, mn in ((0, M1f, "M1"), (1, M2f, "M2")):
            P.op("dve", lambda e, Mf=Mf: e.tensor_tensor(out=prod[:], in0=within[:], in1=Mf, op=ALU.mult), ["within", mn], ["prod"])
            P.op("dve", lambda e, kk=kk: e.tensor_reduce(out=rk[:, kk, :], in_=prod[:], axis=AX.X, op=ALU.add), ["prod"], [f"rk{kk}"])
        counts = carry[:, NA, :]
        P.op("dve", lambda e: e.tensor_tensor(out=cmpb[:], in0=counts.unsqueeze(2).to_broadcast([128, 32, 32]), in1=thr[:, 0:32].unsqueeze(1).to_broadcast([128, 32, 32]), op=ALU.is_gt), ["carry", "cst"], ["cmpb"])
        P.op("dve", lambda e: e.tensor_reduce(out=nb[:], in_=cmpb[:], axis=AX.X, op=ALU.add), ["cmpb"], ["nb"])
        P.op("dve", lambda e: e.tensor_scalar(out=nb[:], in0=nb[:], scalar1=float(MB), scalar2=None, op0=ALU.mult), ["nb"], ["nb"])
        P.op("dve", lambda e: e.tensor_copy(out=endp[:, 0:1], in_=nb[:, 0:1]), ["nb"], ["endp"])
        for ex in range(1, 32):
            P.op("dve", lambda e, ex=ex: e.tensor_tensor(out=endp[:, ex:ex + 1], in0=endp[:, ex - 1:ex], in1=nb[:, ex:ex + 1], op=ALU.add), ["endp", "nb"], ["endp"])
        P.op("dve", lambda e: e.tensor_tensor(out=startp[:], in0=endp[:], in1=nb[:], op=ALU.subtract), ["endp", "nb"], ["startp"])
        for kk, Mf, mn in ((0, M1f, "M1"), (1, M2f, "M2")):
            P.op("dve", lambda e, Mf=Mf: e.tensor_tensor(out=prod[:], in0=Mf, in1=startp[:].unsqueeze(1).to_broadcast([128, NA, 32]), op=ALU.mult), ["startp", mn, f"rk{kk}"], ["prod"])
            P.op("dve", lambda e, kk=kk: e.tensor_reduce(out=destf[:, kk, :], in_=prod[:], axis=AX.X, op=ALU.add), ["prod"], [f"df{kk}"])
            P.op("dve", lambda e, kk=kk: e.tensor_tensor(out=destf[:, kk, :], in0=destf[:, kk, :], in1=rk[:, kk, :], op=ALU.add), [f"df{kk}", f"rk{kk}"], [f"df{kk}"])
        P.op("dve", lambda e: e.tensor_copy(out=desti[:], in_=destf[:]), ["df0", "df1"], ["desti"])
        P.op("dve", lambda e: e.tensor_tensor(out=cmp2[:], in0=endp[:].unsqueeze(1).to_broadcast([128, NBLK, 32]), in1=thr[:, 0:NBLK].unsqueeze(2).to_broadcast([128, NBLK, 32]), op=ALU.is_le), ["endp", "cst"], ["cmp2"])
        P.op("dve", lambda e: e.tensor_reduce(out=bef[:], in_=cmp2[:], axis=AX.X, op=ALU.add), ["cmp2"], ["bef"])
        P.op("dve", lambda e: e.tensor_scalar(out=bef[:], in0=bef[:], scalar1=31.0, scalar2=128.0, op0=ALU.min, op1=ALU.mult), ["bef"], ["bef"])
        P.op("dve", lambda e: e.tensor_scalar(out=bef[:], in0=bef[:], scalar1=iota_p, scalar2=None, op0=ALU.add), ["bef", "cst"], ["bef"])
        P.op("dve", lambda e: e.tensor_copy(out=idxi[:], in_=bef[:]), ["bef"], ["idxi"])

        assert zstate["n"] == NZ, (zstate, NZ)
        hb = [sb(f"hb{i}", [128, D], BF16, at) for i in range(4)]
        for t in range(NA):
            i = t % 4
            P.dma("sp", hb[i][:], h2s[t * 128:(t + 1) * 128, :], reads=[("h2s", t)], writes=[f"hb{i}"], stream=f"ldhb{i}")
            for kk in range(2):
                P.op("pool", lambda e, i=i, kk=kk, t=t: e.indirect_dma_start(out=xs, out_offset=bass.IndirectOffsetOnAxis(ap=desti[:, kk, t:t + 1], axis=0), in_=hb[i][:], in_offset=None),
                     [f"hb{i}", "desti"] + [("xsz", zi) for zi in range(NROWS // 256)], [("xs", t, kk)], stream=f"sc{i}")

        assert cst8["next"] == len(cjobs) and cst8["pend"] is None, (cst8, len(cjobs))
        P.barrier()
        at = [baseB]
        wall = [sb(f"wall{i}", [128, 12288], BF16, at) for i in range(3)]
        xrw = [sb(f"xrw{i}", [128, 2, D], BF16, at) for i in range(2)]
        xTb = [sb(f"xTb{i}", [128, 8, MB], BF16, at) for i in range(2)]
        gsb = [sb(f"gsb{i}", [128, MB], F32, at) for i in range(2)]
        aT = [sb(f"aT{i}", [128, 4, MB], BF16, at) for i in range(2)]
        yo = [sb(f"yo{i}", [128, 2, D], BF16, at) for i in range(2)]
        def load_xrw(b):
            i = b % 2
            P.dma("sp", xrw[i][:], xs[b * MB:(b + 1) * MB, :].rearrange("(j p) d -> p j d", p=128), reads=[("xs", tq, kq) for tq in range(NA) for kq in range(2)], writes=[f"xrw{i}"], stream=f"ldxrw{i}")
        load_xrw(0)
        load_xrw(1)
        for b in range(NBLK):
            i = b % 2
            wi3 = b % 3
            P.op("pool", lambda e, wi3=wi3, b=b: e.indirect_dma_start(out=wall[wi3][:], out_offset=None, in_=wall_bf,
                                                                    in_offset=bass.IndirectOffsetOnAxis(ap=idxi[:, b:b + 1], axis=0)),
                 ["idxi", "wex"], [f"wall{wi3}"], stream=f"gw{wi3}")
            wgv = wall[wi3][:, 0:4096].rearrange("p (k n) -> p k n", k=8)
            wuv = wall[wi3][:, 4096:8192].rearrange("p (k n) -> p k n", k=8)
            wdv = wall[wi3][:, 8192:12288].rearrange("p (k n) -> p k n", k=4)
            for jj in range(2):
                pt, ptn = psT[jj], f"psT{jj}"
                def tr(e, i=i, jj=jj, pt=pt):
                    for k in range(8):
                        ins = e.transpose(out=pt[:, k * 128:(k + 1) * 128], in_=xrw[i][:, jj, k * 128:(k + 1) * 128], identity=ident_bf[:])
                    return ins
                P.op("pe", tr, [f"xrw{i}", "ident"], [ptn])
                if jj == 0:
                    P.op("act", lambda e, pt=pt, i=i, jj=jj: e.copy(out=xTb[i][:, :, jj * 128:(jj + 1) * 128], in_=pt[:].rearrange("p (k t) -> p k t", k=8)), [ptn], [(f"xTb{i}", jj)])
                else:
                    P.op("dve", lambda e, pt=pt, i=i, jj=jj: e.tensor_copy(out=xTb[i][:, :, jj * 128:(jj + 1) * 128], in_=pt[:].rearrange("p (k t) -> p k t", k=8)), [ptn], [(f"xTb{i}", jj)])
            if b + 2 < NBLK:
                load_xrw(b + 2)
            for fo in range(4):
                psg, psgn = next_ps()
                def mg(e, ps=psg, i=i, fo=fo, wgv=wgv, wuv=wuv):
                    for k in range(8):
                        ins = e.matmul(ps[:, 0:MB], lhsT=wgv[:, k, fo * 128:(fo + 1) * 128], rhs=xTb[i][:, k, :], start=(k == 0), stop=(k == 7))
                    for k in range(8):
                        ins = e.matmul(ps[:, MB:2 * MB], lhsT=wuv[:, k, fo * 128:(fo + 1) * 128], rhs=xTb[i][:, k, :], start=(k == 0), stop=(k == 7))
                    return ins
                P.op("pe", mg, [f"wall{wi3}", (f"xTb{i}", 0), (f"xTb{i}", 1)], [psgn])
                gi = fo % 2
                P.op("act", lambda e, ps=psg, gi=gi: e.activation(out=gsb[gi][:], in_=ps[:, 0:MB], func=AF.Silu), [psgn], [f"gsb{gi}"])
                P.op("dve", lambda e, ps=psg, gi=gi, i=i, fo=fo: e.tensor_tensor(out=aT[i][:, fo, :], in0=ps[:, MB:2 * MB], in1=gsb[gi][:], op=ALU.mult), [psgn, f"gsb{gi}"], [(f"aT{i}", fo)])
            for jj in range(2):
                for half in range(2):
                    ps, psn = next_ps()
                    def md(e, ps=ps, i=i, jj=jj, half=half, wdv=wdv):
                        for k in range(4):
                            ins = e.matmul(ps[:], lhsT=aT[i][:, k, jj * 128:(jj + 1) * 128], rhs=wdv[:, k, half * 512:(half + 1) * 512], start=(k == 0), stop=(k == 3))
                        return ins
                    P.op("pe", md, [f"wall{wi3}"] + [(f"aT{i}", k) for k in range(4)], [psn])
                    if half == 0:
                        P.op("act", lambda e, ps=ps, i=i, jj=jj, half=half: e.copy(out=yo[i][:, jj, half * 512:(half + 1) * 512], in_=ps[:]), [psn], [(f"yo{i}", jj, half)])
                    else:
                        P.op("dve", lambda e, ps=ps, i=i, jj=jj, half=half: e.tensor_copy(out=yo[i][:, jj, half * 512:(half + 1) * 512], in_=ps[:]), [psn], [(f"yo{i}", jj, half)])
            P.dma("sp", ys[b * MB:(b + 1) * MB, :].rearrange("(j p) d -> p j d", p=128), yo[i][:], reads=[(f"yo{i}", a, c) for a in range(2) for c in range(2)], writes=[("ys", b)], stream=f"sty{i}")

        P.barrier()
        at = [baseB]
        P.dma("sp", gb[:], g3.partition_broadcast(128), writes=["gb"], stream="m_gb")
        NBUF = 4
        y0 = [sb(f"y0{i}", [128, D], BF16, at) for i in range(NBUF)]
        y1 = [sb(f"y1{i}", [128, D], BF16, at) for i in range(NBUF)]
        xf = [sb(f"xf{i}", [128, D], F32, at) for i in range(NBUF)]
        ot = [sb(f"ot{i}", [128, D], F32, at) for i in range(NBUF)]
        junkf = sb("junkf", [128, D], BF16, at)
        statf = sb("statf", [128, 2 * NBUF], F32, at)

        def cmb_issue(t):
            i = t % NBUF
            P.dma("sp", xf[i][:], x1s[t * 128:(t + 1) * 128, :], reads=[("x1s", t)], writes=[f"xf{i}"], stream=f"ldxf{i}")
            for kk, yb, ybn in ((0, y0, "y0"), (1, y1, "y1")):
                P.op("pool", lambda e, yb=yb, i=i, kk=kk, t=t: e.indirect_dma_start(out=yb[i][:], out_offset=None, in_=ys, in_offset=bass.IndirectOffsetOnAxis(ap=desti[:, kk, t:t + 1], axis=0)),
                     [("ys", bb) for bb in range(NBLK)] + ["desti"], [f"{ybn}{i}"], stream=f"gy{ybn}{i}")

        for t in range(min(NBUF - 1, NA)):
            cmb_issue(t)
        for t in range(NA):
            i = t % NBUF
            s0, s1 = statf[:, 2 * i:2 * i + 1], statf[:, 2 * i + 1:2 * i + 2]
            P.op("dve", lambda e, i=i, t=t: e.scalar_tensor_tensor(out=xf[i][:], in0=y0[i][:], scalar=wk[:, 0, t:t + 1], in1=xf[i][:], op0=ALU.mult, op1=ALU.add), [f"y0{i}", "wk0", f"xf{i}"], [f"xf{i}"])
            P.op("dve", lambda e, i=i, t=t: e.scalar_tensor_tensor(out=xf[i][:], in0=y1[i][:], scalar=wk[:, 1, t:t + 1], in1=xf[i][:], op0=ALU.mult, op1=ALU.add), [f"y1{i}", "wk1", f"xf{i}"], [f"xf{i}"])
            P.op("act", lambda e, i=i, s0=s0: e.activation(out=junkf[:], in_=xf[i][:], func=AF.Square, accum_out=s0), [f"xf{i}"], ["junkf", f"sf0{i}"])
            rstd_ops(s0, s1, [f"sf0{i}"], [f"sf1{i}"])
            P.op("dve", lambda e, i=i, s1=s1: e.scalar_tensor_tensor(out=ot[i][:], in0=xf[i][:], scalar=s1, in1=gb[:], op0=ALU.mult, op1=ALU.mult), [f"xf{i}", f"sf1{i}", "gb"], [f"ot{i}"])
            P.dma("sp", out[t * 128:(t + 1) * 128, :], ot[i][:], reads=[f"ot{i}"], writes=[("out", t)], stream=f"outs{i}")
            if t + NBUF - 1 < NA:
                cmb_issue(t + NBUF - 1)

        P.emit(st, out_streams=[f"outs{i}" for i in range(NBUF)])
    return nc


def make_consts():
    c = np.zeros((128, 6 * 128), np.float32)
    p = np.arange(128)[:, None]
    j = np.arange(128)[None, :]
    c[:, 0:128] = (p == j)
    c[:, 128:256] = (p <= j)
    c[:, 256:384] = (p < j)
    c[:, 384:512] = (p == 127)
    c[:, 512:640] = 1.0
    c[:, 640] = np.arange(128)
    c[:, 641:641 + 96] = 256.0 * np.arange(96)[None, :]
    return c


_CACHE = {}


def run(inputs, n_cores=8, NSEQ=4, debug=False):
    key = (NSEQ, debug)
    if key not in _CACHE:
        _CACHE[key] = build_program(NSEQ, debug)
    nc = _CACHE[key]
    x = np.ascontiguousarray(np.asarray(inputs["x"], dtype=np.float32))
    B = x.shape[0]
    assert B == n_cores * NSEQ
    xs = x.reshape(n_cores, NSEQ * 2048, D)
    shared = {k: np.ascontiguousarray(np.asarray(v, dtype=np.float32)) for k, v in inputs.items() if k != "x"}
    shared["consts"] = make_consts()
    in_maps = []
    for c in range(n_cores):
        m = dict(shared)
        m["x"] = xs[c]
        in_maps.append(m)
    res = run_bass_kernel_spmd(nc, in_maps, core_ids=list(range(n_cores)))
    outs = np.stack([r["out"] for r in res.results], 0).reshape(B, 2048, D)
    if debug:
        return outs, [r for r in res.results]
    return outs


def kernel(**inputs):
    return run(inputs, 8, 4).astype(np.float32)
```
